# Optimizing a Trainium2 kernel written in Bass

```python
import jax, jax.numpy as jnp
from jax import lax
import numpy as np

D_MODEL = 2048
BATCH = 8
SEQ = 2048
DEPTH = 1

D_MIX = D_MODEL
A_HEADS = 8
A_HEAD_DIM = D_MIX // 2 // A_HEADS
A_WIDTH = A_HEADS * A_HEAD_DIM
CHUNK = 128
B_GROUPS = 8
B_WIDTH = D_MIX - A_WIDTH
CONV_W = 3
IN_COLS = 2 * A_WIDTH + 3 * B_WIDTH
N_GROUPS = 4
EXPERTS_PER_GROUP = 8
N_EXPERTS = N_GROUPS * EXPERTS_PER_GROUP
TOP_K = 2
D_EXPERT = D_MODEL // 4
MOE_BLOCK = 128
N_MOD = 6
EPS = 1e-6

kernel_name = "hybrid_gmlp_shortconv_hmoe_adaln"


def rms_norm(x, g):
    xf = x.astype(jnp.float32)
    y = xf * lax.rsqrt(jnp.mean(xf * xf, axis=-1, keepdims=True) + EPS)
    return (y * g.astype(jnp.float32)).astype(x.dtype)


def modulate(h, shift, scale):
    return h * (1 + scale[:, None, :]) + shift[:, None, :]


def gmlp_chunk_mixer(z, w_s, b_s, v_gain):
    bsz, s, _ = z.shape
    u, v = jnp.split(z, 2, axis=-1)
    v = v.reshape(bsz, s // CHUNK, CHUNK, A_HEADS, A_HEAD_DIM)
    v = rms_norm(v, v_gain)
    mask = jnp.tril(jnp.ones((CHUNK, CHUNK), dtype=bool))
    w = jnp.where(mask[None], w_s, jnp.zeros_like(w_s))
    zs = jnp.einsum('htq,bcqhd->bcthd', w, v) + b_s.T[None, None, :, :, None]
    return u * zs.reshape(bsz, s, A_WIDTH)


def short_conv_mixer(z, conv_w, conv_b):
    s = z.shape[1]
    bg, cg, xin = jnp.split(z, 3, axis=-1)
    pre = cg * xin
    p = jnp.pad(pre, ((0, 0), (CONV_W - 1, 0), (0, 0)))
    conv = conv_w[0] * p[:, 0:s]
    for k in range(1, CONV_W):
        conv = conv + conv_w[k] * p[:, k:k + s]
    return bg * (conv + conv_b)


def hier_moe(h, w_router_group, b_router_group, w_router_expert, b_router_expert, w_gate_up, w_down):
    bsz, s, d = h.shape
    t = bsz * s
    xt = h.reshape(t, d)
    xf = xt.astype(jnp.float32)
    g_logits = xf @ w_router_group.astype(jnp.float32) + b_router_group.astype(jnp.float32)
    g_prob = jax.nn.softmax(g_logits, axis=-1)
    grp = jnp.argmax(g_logits, axis=-1)
    p_grp = jnp.take_along_axis(g_prob, grp[:, None], axis=-1)
    e_logits = (xf @ w_router_expert.astype(jnp.float32) + b_router_expert.astype(jnp.float32))
    e_logits = e_logits.reshape(t, N_GROUPS, EXPERTS_PER_GROUP)
    e_logits_g = jnp.take_along_axis(e_logits, grp[:, None, None], axis=1)[:, 0]
    top_val, top_idx = lax.top_k(e_logits_g, TOP_K)
    gates = p_grp * jax.nn.softmax(top_val, axis=-1)
    expert_id = grp[:, None] * EXPERTS_PER_GROUP + top_idx
    n_assign = t * TOP_K
    flat_e = expert_id.reshape(-1).astype(jnp.int32)
    flat_tok = jnp.repeat(jnp.arange(t, dtype=jnp.int32), TOP_K)
    flat_gate = gates.reshape(-1)
    order = jnp.argsort(flat_e)
    se = flat_e[order]
    counts = jnp.bincount(flat_e, length=N_EXPERTS)
    padded = ((counts + MOE_BLOCK - 1) // MOE_BLOCK) * MOE_BLOCK
    starts = jnp.cumsum(counts) - counts
    pends = jnp.cumsum(padded)
    pstarts = pends - padded
    dest = pstarts[se] + (jnp.arange(n_assign, dtype=jnp.int32) - starts[se])
    n_pad = n_assign + N_EXPERTS * MOE_BLOCK
    n_blocks = n_pad // MOE_BLOCK
    buf_tok = jnp.zeros((n_pad,), jnp.int32).at[dest].set(flat_tok[order])
    buf_gate = jnp.zeros((n_pad,), jnp.float32).at[dest].set(flat_gate[order])
    block_expert = jnp.minimum(
        jnp.searchsorted(pends, jnp.arange(n_blocks, dtype=jnp.int32) * MOE_BLOCK, side='right'),
        N_EXPERTS - 1).astype(jnp.int32)
    xb = xt[buf_tok].reshape(n_blocks, MOE_BLOCK, d)

    def expert_block(args):
        xblk, e = args
        gu = xblk @ w_gate_up[e]
        g, u = jnp.split(gu, 2, axis=-1)
        return (jax.nn.silu(g) * u) @ w_down[e]

    yb = lax.map(expert_block, (xb, block_expert))
    y = yb.reshape(n_pad, d) * buf_gate[:, None].astype(h.dtype)
    out = jnp.zeros((t, d), h.dtype).at[buf_tok].add(y)
    return out.reshape(bsz, s, d)


def setup_inputs(seed: int = 0) -> dict:
    key = jax.random.key(seed)
    ks = jax.random.split(key, 24)
    f32 = jnp.float32
    nrm = lambda k, shape, sc: jax.random.normal(k, shape, f32) * sc
    L = DEPTH
    return {
        "x": jax.random.normal(ks[0], (BATCH, SEQ, D_MODEL), f32),
        "c": jax.random.normal(ks[1], (BATCH, D_MODEL), f32),
        "w_ada": nrm(ks[2], (L, D_MODEL, N_MOD * D_MODEL), 0.5 * D_MODEL ** -0.5),
        "b_ada": nrm(ks[3], (L, N_MOD * D_MODEL), 0.02),
        "norm1_g": 1.0 + nrm(ks[4], (L, D_MODEL), 0.02),
        "w_in": nrm(ks[5], (L, D_MODEL, IN_COLS), D_MODEL ** -0.5),
        "w_out": nrm(ks[6], (L, D_MIX, D_MODEL), D_MIX ** -0.5),
        "gmlp_w_s": nrm(ks[7], (L, A_HEADS, CHUNK, CHUNK), CHUNK ** -0.5),
        "gmlp_b_s": 1.0 + nrm(ks[8], (L, A_HEADS, CHUNK), 0.02),
        "gmlp_v_gain": 1.0 + nrm(ks[9], (L, A_HEADS, A_HEAD_DIM), 0.02),
        "conv_w": nrm(ks[10], (L, CONV_W, B_WIDTH), CONV_W ** -0.5),
        "conv_b": nrm(ks[11], (L, B_WIDTH), 0.02),
        "norm2_g": 1.0 + nrm(ks[12], (L, D_MODEL), 0.02),
        "w_router_group": nrm(ks[13], (L, D_MODEL, N_GROUPS), D_MODEL ** -0.5),
        "b_router_group": nrm(ks[14], (L, N_GROUPS), 0.01),
        "w_router_expert": nrm(ks[15], (L, D_MODEL, N_EXPERTS), D_MODEL ** -0.5),
        "b_router_expert": nrm(ks[16], (L, N_EXPERTS), 0.01),
        "w_gate_up": nrm(ks[17], (L, N_EXPERTS, D_MODEL, 2 * D_EXPERT), D_MODEL ** -0.5),
        "w_down": nrm(ks[18], (L, N_EXPERTS, D_EXPERT, D_MODEL), D_EXPERT ** -0.5),
        "w_ada_final": nrm(ks[19], (D_MODEL, 2 * D_MODEL), 0.5 * D_MODEL ** -0.5),
        "b_ada_final": nrm(ks[20], (2 * D_MODEL,), 0.02),
        "norm_f_g": 1.0 + nrm(ks[21], (D_MODEL,), 0.02),
    }


def reference(x, c, w_ada, b_ada, norm1_g, w_in, w_out, gmlp_w_s, gmlp_b_s, gmlp_v_gain,
              conv_w, conv_b, norm2_g, w_router_group, b_router_group, w_router_expert,
              b_router_expert, w_gate_up, w_down, w_ada_final, b_ada_final, norm_f_g):
    c_act = jax.nn.silu(c)
    for l in range(DEPTH):
        mod = c_act @ w_ada[l] + b_ada[l]
        shift1, scale1, gate1, shift2, scale2, gate2 = jnp.split(mod, N_MOD, axis=-1)
        h = modulate(rms_norm(x, norm1_g[l]), shift1, scale1)
        proj = h @ w_in[l]
        y_a = gmlp_chunk_mixer(jax.nn.gelu(proj[..., :2 * A_WIDTH]),
                               gmlp_w_s[l], gmlp_b_s[l], gmlp_v_gain[l])
        y_b = short_conv_mixer(proj[..., 2 * A_WIDTH:], conv_w[l], conv_b[l])
        mix = jnp.concatenate([y_a, y_b], axis=-1) @ w_out[l]
        x = x + gate1[:, None, :] * mix
        h = modulate(rms_norm(x, norm2_g[l]), shift2, scale2)
        ffn = hier_moe(h, w_router_group[l], b_router_group[l], w_router_expert[l],
                       b_router_expert[l], w_gate_up[l], w_down[l])
        x = x + gate2[:, None, :] * ffn
    shift_f, scale_f = jnp.split(c_act @ w_ada_final + b_ada_final, 2, axis=-1)
    return modulate(rms_norm(x, norm_f_g), shift_f, scale_f)
```

```python
import contextlib
import numpy as np
import concourse.bass as bass
import concourse.mybir as mybir
from concourse.bass_utils import run_bass_kernel_spmd

F32 = mybir.dt.float32
BF16 = mybir.dt.bfloat16
I32 = mybir.dt.int32
AF = mybir.ActivationFunctionType
ALU = mybir.AluOpType
AX = mybir.AxisListType
EPS = 1e-6
BIG = 30000.0

REAL = dict(D=2048, S=2048, AW=1024, BW=1024, NG=4, EPG=8, DE=512, C=512, TN=512)


class _Stream:
    def __init__(self, name, eng, sems, inc):
        self.name, self.eng, self.sems, self.inc = name, eng, sems, inc
        self.cnt = [0] * len(sems)
        self.rr = 0


class _Eng:
    def __init__(self, name, h):
        self.name, self.h, self.seen = name, h, {}


class Trk:
    def __init__(self, nc, es, ndma=8):
        mk = lambda n: es.enter_context(nc.semaphore(n))
        self.E = {n: _Eng(n, h) for n, h in [("pe", nc.tensor), ("act", nc.scalar), ("dve", nc.vector),
                                             ("pool", nc.gpsimd), ("sp", nc.sync)]}
        self.S = {}
        for n in ["pe", "act", "dve", "pool"]:
            self.S[n] = _Stream(n, self.E[n], [mk("c_" + n)], 1)
        self.S["sq"] = _Stream("sq", self.E["sp"], [mk(f"sq{i}") for i in range(ndma)], 16)
        self.S["gq"] = _Stream("gq", self.E["pool"], [mk(f"gq{i}") for i in range(ndma)], 16)
        self.lastw = {}
        self.rds = {}
        self.nwait = 0

    def _wait(self, eng, tok):
        st, idx, val = tok
        key = (st.name, idx)
        if eng.seen.get(key, 0) >= val:
            return
        eng.h.wait_ge(st.sems[idx], val)
        eng.seen[key] = val
        self.nwait += 1

    def op(self, sname, fn, r=(), w=()):
        st = self.S[sname]
        eng = st.eng
        deps = []
        for reg in r:
            t = self.lastw.get(reg)
            if t:
                deps.append(t)
        for reg in w:
            t = self.lastw.get(reg)
            if t:
                deps.append(t)
            for (sn, idx), val in self.rds.get(reg, {}).items():
                deps.append((self.S[sn], idx, val))
        if st.inc == 16:
            idx = st.rr
            st.rr = (st.rr + 1) % len(st.sems)
            if st.cnt[idx] > 0:
                deps.append((st, idx, st.cnt[idx]))
        else:
            idx = 0
        for t in deps:
            if t[0] is st and sname == "pe":
                continue
            self._wait(eng, t)
        ins = fn(eng.h)
        st.cnt[idx] += st.inc
        ins.then_inc(st.sems[idx], st.inc)
        tok = (st, idx, st.cnt[idx])
        for reg in w:
            self.lastw[reg] = tok
            self.rds[reg] = {}
        for reg in r:
            if reg not in w:
                self.rds.setdefault(reg, {})[(sname, idx)] = tok[2]
        return tok

    def barrier(self):
        for eng in self.E.values():
            for st in self.S.values():
                for idx, c in enumerate(st.cnt):
                    if c > 0:
                        self._wait(eng, (st, idx, c))
        self.lastw = {}
        self.rds = {}


def build(cfg, gelu_mode="tanh"):
    D, S, AW, BW = cfg["D"], cfg["S"], cfg["AW"], cfg["BW"]
    NG, EPG, DE, C, TN = cfg["NG"], cfg["EPG"], cfg["DE"], cfg["C"], cfg["TN"]
    H = AW // 128
    NE = NG * EPG
    NR = NG + NE
    KD = D // 128
    NT = S // 128
    TPQ = TN // 128
    NQ = S // TN
    KM = (AW + BW) // 128
    ICOLS = 2 * AW + 3 * BW
    BWC = BW // 128
    OGW = min(512, D)
    NOG = D // OGW
    VW = min(512, AW)
    NVG = AW // VW
    HPG = VW // 128
    CGW = min(512, BW)
    NCG = BW // CGW
    CPG = CGW // 128
    B = C // 128
    NEB = NE * B
    NSLOT = NE * C
    DEW = min(512, DE)
    NDG = DE // DEW
    KE = DE // 128
    KMAX = max(KD, KM)
    CWA = min(512, D)
    NCA = D // CWA
    KPC = CWA // 128
    assert VW == 512 and CGW == 512

    nc = bass.Bass("TRN2", target_bir_lowering=False)

    def din(name, shape, dt=F32):
        return nc.dram_tensor(name, list(shape), dt, kind="ExternalInput").ap()

    x_d = din("x", [S, D])
    c_d = din("c_pk", [128, KD])
    w_ada_d = din("w_ada", [6 * NCA, 128, KD, CWA])
    b_ada_d = din("b_ada", [6 * D])
    n1g_d = din("norm1_g", [D])
    w_in_d = din("w_in", [ICOLS // 512, 128, KD, 512])
    w_out_d = din("w_out", [NOG, 128, KM, OGW])
    wsT_d = din("wsT", [128, H * 128])
    bs_d = din("b_s", [H * 128])
    vg_d = din("v_gain", [AW])
    cw_d = din("conv_w_pk", [128, 3 * BWC])
    cbias_d = din("conv_b_pk", [128, BWC])
    n2g_d = din("norm2_g", [D])
    wr_d = din("wr", [D, NR])
    rb_d = din("rb", [NR])
    wgu_d = din("w_gate_up", [NE, 128, KD, 2 * DE])
    wdn_d = din("w_down", [NE, 128, KE, D])
    w_adaf_d = din("w_ada_final", [2 * NCA, 128, KD, CWA])
    b_adaf_d = din("b_ada_final", [2 * D])
    nfg_d = din("norm_f_g", [D])
    ident_d = din("ident", [128, 128])
    triu_d = din("triu", [128, 128])
    triu0_d = din("triu0", [128, 128])
    ecst_d = din("ecst", [128, NE])
    tokid_d = din("tokid", [128, NT], I32)
    out_d = nc.dram_tensor("out", [S, D], F32, kind="ExternalOutput").ap()
    x1_d = nc.dram_tensor("x1_scr", [S, D], F32, kind="Internal").ap()
    h2_d = nc.dram_tensor("h2_scr", [S, D], BF16, kind="Internal").ap()
    y_d = nc.dram_tensor("y_scr", [NSLOT, D], BF16, kind="Internal").ap()
    tki_d = nc.dram_tensor("tki_scr", [128 * NEB, 1], I32, kind="Internal").ap()
    modv_d = nc.dram_tensor("modv_scr", [3, 128, D], F32, kind="Internal").ap()

    es = contextlib.ExitStack()
    with es:
        T = Trk(nc, es)
        _uid = [0]

        def sb(name, shape, dt=F32, stack=es):
            _uid[0] += 1
            return stack.enter_context(nc.sbuf_tensor(f"{name}_{_uid[0]}", list(shape), dt))
        identf = sb("identf", [128, 128])
        identb = sb("identb", [128, 128], BF16)
        triub = sb("triub", [128, 128], BF16)
        onesb = sb("onesb", [128, 128], BF16)
        ecst = sb("ecst_t", [128, NE])
        tokid = sb("tokid_t", [128, NT], I32)
        wmT = sb("wmT", [128, H, 128], BF16)
        bsb = sb("bsb", [128, H, 128])
        gainb = sb("gainb", [128, AW])
        cw = sb("cw", [128, 3, BWC])
        cbias = sb("cbias", [128, BWC])
        wrb = sb("wrb", [128, KD, NR], BF16)
        rbb = sb("rbb", [128, NR])
        cact = sb("cact", [128, KD])
        cb = sb("cb", [128, KD, 128], BF16)
        gv1p = sb("gv1p", [128, KD])
        sh1p = sb("sh1p", [128, KD])
        gv2p = sb("gv2p", [128, KD])
        sh2p = sb("sh2p", [128, KD])
        A_all = sb("A_all", [128, NT, NE], BF16)
        slots_i = sb("slots_i", [128, NT, 2], I32)
        gates = sb("gates", [128, NT, 2])
        tokidx = sb("tokidx", [128, NEB], I32)
        carry = sb("carry", [128, BWC, 2])
        zt = sb("zt", [128, NEB], I32)
        sm = sb("sm", [128, 64])
        smi = sb("smi", [128, 16], I32)
        LNEB = NEB.bit_length() - 1
        assert (1 << LNEB) == NEB
        psT = es.enter_context(nc.psum_tensor("psT", [128, max(KD * 128, KE * 128, 1024)], BF16))
        NB = 6
        psb = [es.enter_context(nc.psum_tensor(f"ps{i}", [128, 512], F32)) for i in range(NB)]
        bank_rr = [0]

        def bank():
            i = bank_rr[0]
            bank_rr[0] = (i + 1) % NB
            return psb[i], f"ps{i}"

        op = T.op

        op("sq", lambda e: e.dma_start(out=identf[:], in_=ident_d), w=["identf"])
        op("gq", lambda e: e.dma_start(out=identb[:], in_=ident_d), w=["identb"])
        op("gq", lambda e: e.dma_start(out=triub[:], in_=triu_d), w=["triub"])
        op("sq", lambda e: e.dma_start(out=ecst[:], in_=ecst_d), w=["ecst"])
        op("sq", lambda e: e.dma_start(out=tokid[:], in_=tokid_d), w=["tokid"])
        with contextlib.ExitStack() as s0:
            wsf = sb("wsf", [128, H, 128], F32, s0)
            mk0 = sb("mk0", [128, 128], F32, s0)
            op("sq", lambda e: e.dma_start(out=wsf[:].rearrange("p h t -> p (h t)"), in_=wsT_d), w=["wsf"])
            op("sq", lambda e: e.dma_start(out=mk0[:], in_=triu0_d), w=["mk0"])
            op("dve", lambda e: e.tensor_tensor(out=wmT[:], in0=wsf[:], in1=mk0[:].unsqueeze(1).to_broadcast([128, H, 128]), op=ALU.mult),
               r=["wsf", "mk0"], w=["wmT"])
            T.barrier()
        op("sq", lambda e: e.dma_start(out=bsb[:].rearrange("p h t -> p (h t)"), in_=bs_d.partition_broadcast(128)), w=["bsb"])
        op("sq", lambda e: e.dma_start(out=gainb[:], in_=vg_d.partition_broadcast(128)), w=["gainb"])
        op("sq", lambda e: e.dma_start(out=cw[:].rearrange("p a b -> p (a b)"), in_=cw_d), w=["cw"])
        op("sq", lambda e: e.dma_start(out=cbias[:], in_=cbias_d), w=["cbias"])
        op("gq", lambda e: e.dma_start(out=wrb[:], in_=wr_d.rearrange("(k p) n -> p k n", p=128)), w=["wrb"])
        op("sq", lambda e: e.dma_start(out=rbb[:], in_=rb_d.partition_broadcast(128)), w=["rbb"])
        op("sq", lambda e: e.dma_start(out=cact[:], in_=c_d), w=["cact"])
        op("dve", lambda e: e.memset(onesb[:], 1.0), w=["onesb"])
        op("dve", lambda e: e.memset(carry[:], 0.0), w=["carry"])
        op("dve", lambda e: e.memset(zt[:], S), w=["zt"])
        op("sq", lambda e: e.dma_start(out=tki_d.rearrange("(p n) o -> p (n o)", p=128), in_=zt[:]), r=["zt"], w=["tki_d"])
        op("act", lambda e: e.activation(out=cact[:], in_=cact[:], func=AF.Silu), r=["cact"], w=["cact"])
        for k in range(KD):
            op("dve", lambda e, k=k: e.tensor_scalar(out=cb[:, k, :], in0=onesb[:], scalar1=cact[:, k:k + 1], scalar2=None,
                                                     op0=ALU.mult), r=["cact", "onesb"], w=["cb"])

        def rstd_from_ss(ss_ap, rs_ap, n, regs_r, regs_w):
            op("dve", lambda e: e.tensor_scalar(out=rs_ap, in0=ss_ap, scalar1=1.0 / n, scalar2=EPS, op0=ALU.mult, op1=ALU.add),
               r=regs_r, w=regs_w)
            op("act", lambda e: e.activation(out=rs_ap, in_=rs_ap, func=AF.Sqrt), r=regs_w, w=regs_w)
            op("dve", lambda e: e.reciprocal(out=rs_ap, in_=rs_ap), r=regs_w, w=regs_w)

        def gelu(out_ap, in_ap, tmp_ap, r, w, wt):
            if gelu_mode == "tanh":
                op("act", lambda e: e.activation(out=out_ap, in_=in_ap, func=AF.Gelu_apprx_tanh), r=r, w=w)
            else:
                op("act", lambda e: e.activation(out=tmp_ap, in_=in_ap, func=AF.Square), r=r, w=wt)
                op("dve", lambda e: e.tensor_scalar(out=tmp_ap, in0=tmp_ap, scalar1=0.044715 * 1.5957691216, scalar2=1.5957691216,
                                                    op0=ALU.mult, op1=ALU.add), r=wt, w=wt)
                op("dve", lambda e: e.tensor_tensor(out=tmp_ap, in0=tmp_ap, in1=in_ap, op=ALU.mult), r=list(wt) + list(r), w=wt)
                op("act", lambda e: e.activation(out=tmp_ap, in_=tmp_ap, func=AF.Sigmoid), r=wt, w=wt)
                op("dve", lambda e: e.tensor_tensor(out=out_ap, in0=tmp_ap, in1=in_ap, op=ALU.mult), r=list(wt) + list(r), w=w)

        def transpose_mod(src_ap, src_reg, sc_pp, sc_reg, bi_pp, bi_reg, dst_ap, dst_reg):
            for k in range(KD):
                op("pe", lambda e, k=k: e.transpose(out=psT[:, k * 128:(k + 1) * 128], in_=src_ap[:, k * 128:(k + 1) * 128],
                                                    identity=identb[:]), r=[src_reg, "identb"], w=["psT"])
            mt = modtmp_box[0]
            op("dve", lambda e: e.tensor_tensor(out=mt[:].rearrange("p (k t) -> p k t", t=128),
                                                in0=psT[:, :KD * 128].rearrange("p (k t) -> p k t", t=128),
                                                in1=sc_pp[:, :].unsqueeze(2).to_broadcast([128, KD, 128]), op=ALU.mult),
               r=["psT", sc_reg], w=["modtmp"])
            op("dve", lambda e: e.tensor_tensor(out=dst_ap, in0=mt[:].rearrange("p (k t) -> p k t", t=128),
                                                in1=bi_pp[:, :].unsqueeze(2).to_broadcast([128, KD, 128]), op=ALU.add),
               r=["modtmp", bi_reg], w=[dst_reg])

        modtmp_box = [None]

        def ada_vec(stack_tiles, wsrc, bsrc, col0, out_ap, out_reg):
            wa, bb = stack_tiles
            for ci in range(NCA):
                j = ada_rr[0]
                ada_rr[0] = (j + 1) % 2
                cs = slice(col0 + ci * CWA, col0 + (ci + 1) * CWA)
                op("gq", lambda e: e.dma_start(out=wa[j][:, :, :], in_=wsrc[(col0 // CWA) + ci]), w=[f"wa{j}"])
                op("sq", lambda e: e.dma_start(out=bb[j][:, :], in_=bsrc[cs].partition_broadcast(128)), w=[f"bb{j}"])
                ps, pr = bank()
                for k in range(KD):
                    op("pe", lambda e, k=k: e.matmul(ps[:, :CWA], lhsT=cb[:, k, :], rhs=wa[j][:, k, :], start=(k == 0), stop=(k == KD - 1)),
                       r=["cb", f"wa{j}"], w=[pr])
                op("dve", lambda e: e.tensor_tensor(out=out_ap[:, ci * CWA:(ci + 1) * CWA], in0=ps[:, :CWA], in1=bb[j][:, :], op=ALU.add),
                   r=[pr, f"bb{j}"], w=[out_reg])

        ada_rr = [0]

        def gain_mul(vec_ap, vec_reg, gsrc, tmp_ap, tmp_reg):
            op("sq", lambda e: e.dma_start(out=tmp_ap, in_=gsrc.partition_broadcast(128)), w=[tmp_reg])
            op("dve", lambda e: e.scalar_tensor_tensor(out=vec_ap, in0=vec_ap, scalar=1.0, in1=tmp_ap, op0=ALU.add, op1=ALU.mult),
               r=[vec_reg, tmp_reg], w=[vec_reg])

        def to_pp(vec_ap, vec_reg, pp_ap, pp_reg, tmp_ap, tmp_reg):
            op("dve", lambda e: e.tensor_tensor(out=tmp_ap.rearrange("p (k j) -> p k j", j=128),
                                                in0=vec_ap.rearrange("p (k j) -> p k j", j=128),
                                                in1=identf[:].unsqueeze(1).to_broadcast([128, KD, 128]), op=ALU.mult),
               r=[vec_reg, "identf"], w=[tmp_reg])
            op("dve", lambda e: e.tensor_reduce(out=pp_ap, in_=tmp_ap.rearrange("p (k j) -> p k j", j=128), axis=AX.X, op=ALU.add),
               r=[tmp_reg], w=[pp_reg])

        gate1b = sb("gate1b", [128, D])
        with contextlib.ExitStack() as s1:
            wa = [sb(f"wa{j}", [128, KD, CWA], BF16, s1) for j in range(2)]
            bb = [sb(f"bb{j}", [128, CWA], F32, s1) for j in range(2)]
            va = sb("ada_va", [128, D], F32, s1)
            vb = sb("ada_vb", [128, D], F32, s1)
            ada_vec((wa, bb), w_ada_d, b_ada_d, 0 * D, va[:], "ada_va")
            to_pp(va[:], "ada_va", sh1p[:], "sh1p", vb[:], "ada_vb")
            ada_vec((wa, bb), w_ada_d, b_ada_d, 1 * D, va[:], "ada_va")
            gain_mul(va[:], "ada_va", n1g_d, vb[:], "ada_vb")
            to_pp(va[:], "ada_va", gv1p[:], "gv1p", vb[:], "ada_vb")
            T.barrier()

        with contextlib.ExitStack() as s2:
            NWB = 3
            wbig = [sb(f"wbig{j}", [128, KMAX, 512], BF16, s2) for j in range(NWB)]
            wrr = [0]

            def load_w(src, nk, ncols):
                j = wrr[0]
                wrr[0] = (j + 1) % NWB
                op("gq", lambda e: e.dma_start(out=wbig[j][:, :nk, :ncols], in_=src), w=[f"wbig{j}"])
                return wbig[j], f"wbig{j}"

            h1T = sb("h1T", [128, KD, TN], BF16, s2)
            vn = sb("vn", [128, TPQ, AW], BF16, s2)
            yT = sb("yT", [128, KM, TN], BF16, s2)
            xq = sb("xq", [128, TPQ, D], F32, s2)
            xn = [sb(f"xn{j}", [128, D], BF16, s2) for j in range(2)]
            h2T = sb("h2T", [128, KD, 128], BF16, s2)
            vt = [sb(f"vt{j}", [128, VW], F32, s2) for j in range(2)]
            vsq = [sb(f"vsq{j}", [128, VW], F32, s2) for j in range(2)]
            uT = [sb(f"uT{j}", [128, TN], BF16, s2) for j in range(2)]
            gtmp = [sb(f"gtmp{j}", [128, 512], F32, s2) for j in range(2)] if gelu_mode != "tanh" else None
            gt = lambda j, n: gtmp[j][:, :n] if gtmp is not None else None
            modtmp_box[0] = sb("modtmp", [128, KD * 128], F32, s2)
            _ztmp1 = sb("ztmp0", [128, TN], F32, s2)
            ztmp = [_ztmp1, _ztmp1]
            cT = sb("cT", [128, CPG, TN], F32, s2)
            pre = sb("pre", [128, CPG, TN + 2], F32, s2)
            _cv1 = sb("cv0", [128, TN], F32, s2)
            cv = [_cv1, _cv1]
            _otmp1 = sb("otmp0", [128, OGW], F32, s2)
            otmp = [_otmp1, _otmp1]
            rt4 = [sb(f"rt4_{i}", [128, TPQ, NE], F32, s2) for i in range(10)]
            smr = sb("smr", [128, 32], F32, s2)
            smr2 = sb("smr2", [128, 11 * TPQ], F32, s2)
            smi8 = [sb(f"smi8_{i}", [128, 6 * TPQ], I32, s2) for i in range(2)]
            rtg = [sb(f"rtg_{i}", [128, TPQ, NG], F32, s2) for i in range(4)]
            cntq = sb("cntq", [128, NE], F32, s2)
            op("dve", lambda e: e.memset(cntq[:], 0.0), w=["cntq"])

            ada_q = []
            for kind, wsrc, bsrc, col0, gsrc in (("gate1", w_ada_d, b_ada_d, 2 * D, None), ("sh2", w_ada_d, b_ada_d, 3 * D, None),
                                                 ("gv2", w_ada_d, b_ada_d, 4 * D, n2g_d), ("gate2", w_ada_d, b_ada_d, 5 * D, None),
                                                 ("shf", w_adaf_d, b_adaf_d, 0, None), ("gvf", w_adaf_d, b_adaf_d, D, nfg_d)):
                for ci in range(NCA):
                    ada_q.append((kind, wsrc, bsrc, col0, gsrc, ci))
            N_EARLY = 3 * NCA

            def ada_load(spec):
                kind, wsrc, bsrc, col0, gsrc, ci = spec
                wa_, war = load_w(wsrc[(col0 // CWA) + ci], KD, CWA)
                return spec, wa_, war

            def ada_chunk(spec):
                ada_compute(*ada_load(spec))

            ada_pending = [None]

            def pump_tail(last_in_quarter):
                if ada_pending[0] is not None:
                    ada_compute(*ada_pending[0])
                    ada_pending[0] = None
                if not last_in_quarter and ada_done[0] < len(ada_q):
                    ada_pending[0] = ada_load(ada_q[ada_done[0]])
                    ada_done[0] += 1

            def ada_compute(spec, wa_, war):
                kind, wsrc, bsrc, col0, gsrc, ci = spec
                cs = slice(col0 + ci * CWA, col0 + (ci + 1) * CWA)
                ls = slice(ci * CWA, (ci + 1) * CWA)
                bbm, gbm, adt, adt2 = vt[0][:, :CWA], vt[1][:, :CWA], vsq[0][:, :CWA], vsq[1][:, :CWA]
                op("sq", lambda e: e.dma_start(out=bbm, in_=bsrc[cs].partition_broadcast(128)), w=["vt0"])
                ps, pr = bank()
                for k in range(KD):
                    op("pe", lambda e, k=k: e.matmul(ps[:, :CWA], lhsT=cb[:, k, :], rhs=wa_[:, k, :CWA], start=(k == 0), stop=(k == KD - 1)),
                       r=["cb", war], w=[pr])
                if kind == "gate1":
                    op("dve", lambda e: e.tensor_tensor(out=gate1b[:, ls], in0=ps[:, :CWA], in1=bbm, op=ALU.add), r=[pr, "vt0"], w=["gate1b"])
                    return
                op("dve", lambda e: e.tensor_tensor(out=adt, in0=ps[:, :CWA], in1=bbm, op=ALU.add), r=[pr, "vt0"], w=["vsq0"])
                if gsrc is not None:
                    op("sq", lambda e: e.dma_start(out=gbm, in_=gsrc[ls].partition_broadcast(128)), w=["vt1"])
                    op("dve", lambda e: e.scalar_tensor_tensor(out=adt, in0=adt, scalar=1.0, in1=gbm, op0=ALU.add, op1=ALU.mult),
                       r=["vsq0", "vt1"], w=["vsq0"])
                if kind in ("sh2", "gv2"):
                    pp_t, pp_r = (sh2p, "sh2p") if kind == "sh2" else (gv2p, "gv2p")
                    op("dve", lambda e: e.tensor_tensor(out=adt2.rearrange("p (k j) -> p k j", j=128),
                                                        in0=adt.rearrange("p (k j) -> p k j", j=128),
                                                        in1=identf[:].unsqueeze(1).to_broadcast([128, KPC, 128]), op=ALU.mult),
                       r=["vsq0", "identf"], w=["vsq1"])
                    op("dve", lambda e: e.tensor_reduce(out=pp_t[:, ci * KPC:(ci + 1) * KPC], in_=adt2.rearrange("p (k j) -> p k j", j=128),
                                                        axis=AX.X, op=ALU.add), r=["vsq1"], w=[pp_r])
                else:
                    v = {"gate2": 0, "shf": 1, "gvf": 2}[kind]
                    op("sq", lambda e: e.dma_start(out=modv_d[v, :, ls], in_=adt), r=["vsq0"], w=["modv_d"])

            ada_done = [0]

            def pump_ada(n, upto=None):
                lim = len(ada_q) if upto is None else upto
                while n > 0 and ada_done[0] < lim:
                    ada_chunk(ada_q[ada_done[0]])
                    ada_done[0] += 1
                    n -= 1

            for q in range(NQ):
                for tt in range(TPQ):
                    ti = q * TPQ + tt
                    xr = f"xq{tt}"
                    op("sq", lambda e: e.dma_start(out=xq[:, tt, :], in_=x_d[ti * 128:(ti + 1) * 128, :]), w=[xr])
                    j = ti % 2
                    ssA = sm[:, 0:1]
                    op("dve", lambda e: e.memset(ssA, 0.0), w=["ssA"])
                    op("act", lambda e: e.activation(out=xn[j][:], in_=xq[:, tt, :], func=AF.Square, accum_out=ssA),
                       r=[xr, "ssA"], w=[f"xn{j}", "ssA"])
                    rstd_from_ss(ssA, sm[:, 1:2], D, ["ssA"], ["rsA"])
                    op("dve", lambda e: e.tensor_scalar(out=xn[j][:], in0=xq[:, tt, :], scalar1=sm[:, 1:2], scalar2=None, op0=ALU.mult),
                       r=[xr, "rsA"], w=[f"xn{j}"])
                    transpose_mod(xn[j], f"xn{j}", gv1p, "gv1p", sh1p, "sh1p", h1T[:, :, tt * 128:(tt + 1) * 128], "h1T")
                for vg in range(NVG):
                    wv, wvr = load_w(w_in_d[(AW + vg * VW) // 512], KD, VW)
                    for tt in range(TPQ):
                        ps, pr = bank()
                        for k in range(KD):
                            op("pe", lambda e, k=k: e.matmul(ps[:, :VW], lhsT=h1T[:, k, tt * 128:(tt + 1) * 128], rhs=wv[:, k, :VW],
                                                             start=(k == 0), stop=(k == KD - 1)), r=["h1T", wvr], w=[pr])
                        j = tt % 2
                        gelu(vt[j][:], ps[:, :VW], gt(j, VW), [pr], [f"vt{j}"], [f"gtmp{j}"])
                        op("dve", lambda e: e.tensor_tensor(out=vsq[j][:], in0=vt[j][:], in1=vt[j][:], op=ALU.mult), r=[f"vt{j}"], w=[f"vsq{j}"])
                        vss = smr[:, 0:HPG]
                        op("dve", lambda e: e.tensor_reduce(out=vss, in_=vsq[j][:].rearrange("p (h d) -> p h d", d=128), axis=AX.X, op=ALU.add),
                           r=[f"vsq{j}"], w=["vss"])
                        rstd_from_ss(vss, smr[:, 8:8 + HPG], 128, ["vss"], ["vrs"])
                        op("dve", lambda e: e.tensor_tensor(out=vsq[j][:].rearrange("p (h d) -> p h d", d=128),
                                                            in0=vt[j][:].rearrange("p (h d) -> p h d", d=128),
                                                            in1=smr[:, 8:8 + HPG].unsqueeze(2).to_broadcast([128, HPG, 128]), op=ALU.mult),
                           r=[f"vt{j}", "vrs"], w=[f"vsq{j}"])
                        op("dve", lambda e: e.tensor_tensor(out=vn[:, tt, vg * VW:(vg + 1) * VW], in0=vsq[j][:],
                                                            in1=gainb[:, vg * VW:(vg + 1) * VW], op=ALU.mult),
                           r=[f"vsq{j}", "gainb"], w=["vn"])
                for ug in range(NVG):
                    pump_ada(2, upto=N_EARLY)
                    wu, wur = load_w(w_in_d[(ug * VW) // 512], KD, VW)
                    for hh in range(HPG):
                        h = ug * HPG + hh
                        ps, pr = bank()
                        for k in range(KD):
                            op("pe", lambda e, k=k: e.matmul(ps[:, :TN], lhsT=wu[:, k, hh * 128:(hh + 1) * 128], rhs=h1T[:, k, :],
                                                             start=(k == 0), stop=(k == KD - 1)), r=["h1T", wur], w=[pr])
                        j = h % 2
                        gelu(uT[j][:], ps[:, :TN], gt(j, TN), [pr], [f"uT{j}"], [f"gtmp{j}"])
                        ps2, pr2 = bank()
                        for c in range(TPQ):
                            op("pe", lambda e, c=c: e.matmul(ps2[:, c * 128:(c + 1) * 128], lhsT=vn[:, c, h * 128:(h + 1) * 128],
                                                             rhs=wmT[:, h, :], start=True, stop=True), r=["vn", "wmT"], w=[pr2])
                        op("dve", lambda e: e.tensor_tensor(out=ztmp[j][:].rearrange("p (c t) -> p c t", t=128),
                                                            in0=ps2[:, :TN].rearrange("p (c t) -> p c t", t=128),
                                                            in1=bsb[:, h, :].unsqueeze(1).to_broadcast([128, TPQ, 128]), op=ALU.add),
                           r=[pr2, "bsb"], w=["ztmp0"])
                        op("dve", lambda e: e.tensor_tensor(out=yT[:, h, :], in0=ztmp[j][:], in1=uT[j][:], op=ALU.mult),
                           r=["ztmp0", f"uT{j}"], w=[f"yT{h}"])
                for cg in range(NCG):
                    base = 2 * AW
                    pump_ada(2, upto=N_EARLY)
                    wc, wcr = load_w(w_in_d[(base + BW + cg * CGW) // 512], KD, CGW)
                    for jx in range(CPG):
                        ps, pr = bank()
                        for k in range(KD):
                            op("pe", lambda e, k=k: e.matmul(ps[:, :TN], lhsT=wc[:, k, jx * 128:(jx + 1) * 128], rhs=h1T[:, k, :],
                                                             start=(k == 0), stop=(k == KD - 1)), r=["h1T", wcr], w=[pr])
                        op("act", lambda e: e.copy(out=cT[:, jx, :], in_=ps[:, :TN]), r=[pr], w=[f"cT{jx}"])
                    pump_ada(2, upto=N_EARLY)
                    wx, wxr = load_w(w_in_d[(base + 2 * BW + cg * CGW) // 512], KD, CGW)
                    for jx in range(CPG):
                        jj = cg * CPG + jx
                        ps, pr = bank()
                        for k in range(KD):
                            op("pe", lambda e, k=k: e.matmul(ps[:, :TN], lhsT=wx[:, k, jx * 128:(jx + 1) * 128], rhs=h1T[:, k, :],
                                                             start=(k == 0), stop=(k == KD - 1)), r=["h1T", wxr], w=[pr])
                        op("dve", lambda e: e.tensor_copy(out=pre[:, jx, 0:2], in_=carry[:, jj, :]), r=["carry"], w=[f"pre{jx}"])
                        op("dve", lambda e: e.tensor_tensor(out=pre[:, jx, 2:TN + 2], in0=ps[:, :TN], in1=cT[:, jx, :], op=ALU.mult),
                           r=[pr, f"cT{jx}"], w=[f"pre{jx}"])
                        op("dve", lambda e: e.tensor_copy(out=carry[:, jj, :], in_=pre[:, jx, TN:TN + 2]), r=[f"pre{jx}"], w=["carry"])
                    pump_ada(2, upto=N_EARLY)
                    wb, wbr = load_w(w_in_d[(base + cg * CGW) // 512], KD, CGW)
                    for jx in range(CPG):
                        jj = cg * CPG + jx
                        j = jj % 2
                        ps, pr = bank()
                        for k in range(KD):
                            op("pe", lambda e, k=k: e.matmul(ps[:, :TN], lhsT=wb[:, k, jx * 128:(jx + 1) * 128], rhs=h1T[:, k, :],
                                                             start=(k == 0), stop=(k == KD - 1)), r=["h1T", wbr], w=[pr])
                        op("dve", lambda e: e.tensor_scalar(out=cv[j][:], in0=pre[:, jx, 0:TN], scalar1=cw[:, 0, jj:jj + 1], scalar2=None,
                                                            op0=ALU.mult), r=[f"pre{jx}", "cw"], w=["cv0"])
                        for kk in (1, 2):
                            op("dve", lambda e, kk=kk: e.scalar_tensor_tensor(out=cv[j][:], in0=pre[:, jx, kk:kk + TN],
                                                                              scalar=cw[:, kk, jj:jj + 1], in1=cv[j][:],
                                                                              op0=ALU.mult, op1=ALU.add),
                               r=[f"pre{jx}", "cw", "cv0"], w=["cv0"])
                        op("dve", lambda e: e.scalar_tensor_tensor(out=yT[:, H + jj, :], in0=cv[j][:], scalar=cbias[:, jj:jj + 1],
                                                                   in1=ps[:, :TN], op0=ALU.add, op1=ALU.mult),
                           r=["cv0", "cbias", pr], w=[f"yT{H + jj}"])
                pump_ada(99, upto=N_EARLY)
                yregs = [f"yT{k}" for k in range(KM)]
                for og in range(NOG):
                    wo, wor = load_w(w_out_d[og], KM, OGW)
                    for tt in range(TPQ):
                        ps, pr = bank()
                        for k in range(KM):
                            op("pe", lambda e, k=k: e.matmul(ps[:, :OGW], lhsT=yT[:, k, tt * 128:(tt + 1) * 128], rhs=wo[:, k, :OGW],
                                                             start=(k == 0), stop=(k == KM - 1)), r=[yregs[k], wor], w=[pr])
                        j = tt % 2
                        xr = f"xq{tt}"
                        op("dve", lambda e: e.tensor_tensor(out=otmp[j][:], in0=ps[:, :OGW], in1=gate1b[:, og * OGW:(og + 1) * OGW], op=ALU.mult),
                           r=[pr, "gate1b"], w=["otmp0"])
                        op("dve", lambda e: e.tensor_tensor(out=xq[:, tt, og * OGW:(og + 1) * OGW], in0=otmp[j][:],
                                                            in1=xq[:, tt, og * OGW:(og + 1) * OGW], op=ALU.add),
                           r=["otmp0", xr], w=[xr])
                ti0 = q * TPQ
                T4 = TPQ
                ssq, rsq = smr[:, 0:T4], smr[:, 4:4 + T4]
                RT = ["rt"]
                op("dve", lambda e: e.memset(ssq, 0.0), w=["ssq"])
                for tt in range(TPQ):
                    ti = ti0 + tt
                    xr = f"xq{tt}"
                    pump_tail(tt == TPQ - 1)
                    op("sq", lambda e: e.dma_start(out=x1_d[ti * 128:(ti + 1) * 128, :], in_=xq[:, tt, :]), r=[xr], w=["x1_d"])
                    op("act", lambda e: e.activation(out=xn[tt % 2][:], in_=xq[:, tt, :], func=AF.Square, accum_out=ssq[:, tt:tt + 1]),
                       r=[xr, "ssq"], w=[f"xn{tt % 2}", "ssq"])
                rstd_from_ss(ssq, rsq, D, ["ssq"], ["rsq"])
                psL, prL = bank()
                for tt in range(TPQ):
                    ti = ti0 + tt
                    xr = f"xq{tt}"
                    j = tt % 2
                    op("dve", lambda e: e.tensor_scalar(out=xn[j][:], in0=xq[:, tt, :], scalar1=rsq[:, tt:tt + 1], scalar2=None, op0=ALU.mult),
                       r=[xr, "rsq"], w=[f"xn{j}"])
                    transpose_mod(xn[j], f"xn{j}", gv2p, "gv2p", sh2p, "sh2p", h2T[:, :, :], "h2T")
                    for k in range(KD):
                        op("pe", lambda e, k=k: e.matmul(psL[:, tt * NR:(tt + 1) * NR], lhsT=h2T[:, k, :], rhs=wrb[:, k, :],
                                                         start=(k == 0), stop=(k == KD - 1)), r=["h2T", "wrb"], w=[prL])
                    for k in range(KD):
                        op("pe", lambda e, k=k: e.transpose(out=psT[:, k * 128:(k + 1) * 128], in_=h2T[:, k, :], identity=identb[:]),
                           r=["h2T", "identb"], w=["psT"])
                    op("act", lambda e: e.copy(out=xn[j][:], in_=psT[:, :KD * 128]), r=["psT"], w=[f"xn{j}"])
                    op("sq", lambda e: e.dma_start(out=h2_d[ti * 128:(ti + 1) * 128, :], in_=xn[j][:]), r=[f"xn{j}"], w=["h2_d"])
                dv = lambda f, r_=(): op("dve", f, r=RT + list(r_), w=RT)
                glt, elt = rtg[0][:, :, :], rt4[1][:, :, :]
                psL3 = psL[:, :T4 * NR].rearrange("p (t n) -> p t n", n=NR)
                rbb3 = rbb[:, :].unsqueeze(1).to_broadcast([128, T4, NR])
                dv(lambda e: e.tensor_tensor(out=glt, in0=psL3[:, :, 0:NG], in1=rbb3[:, :, 0:NG], op=ALU.add), [prL, "rbb"])
                dv(lambda e: e.tensor_tensor(out=elt, in0=psL3[:, :, NG:NR], in1=rbb3[:, :, NG:NR], op=ALU.add), [prL, "rbb"])
                gmax, sume, pgr, m1, m2, dd, e2, ga, gb, s0f, s1f = (smr2[:, i * T4:(i + 1) * T4] for i in range(11))
                ohg, pen, eg = rtg[1][:, :, :], rtg[2][:, :, :], rtg[3][:, :, :]
                mm, oh1, mm2, oh2, slf, tmpe = rt4[5][:, :, :], rt4[6][:, :, :], rt4[7][:, :, :], rt4[8][:, :, :], rt4[9][:, :, :], rt4[2][:, :, :]
                b3 = lambda a, n: a.unsqueeze(2).to_broadcast([128, T4, n])
                dv(lambda e: e.tensor_reduce(out=gmax, in_=glt, axis=AX.X, op=ALU.max))
                dv(lambda e: e.tensor_tensor(out=ohg, in0=glt, in1=b3(gmax, NG), op=ALU.is_equal))
                dv(lambda e: e.tensor_tensor(out=eg, in0=glt, in1=b3(gmax, NG), op=ALU.subtract))
                op("act", lambda e: e.activation(out=eg, in_=eg, func=AF.Exp), r=RT, w=RT)
                dv(lambda e: e.tensor_reduce(out=sume, in_=eg, axis=AX.X, op=ALU.add))
                dv(lambda e: e.reciprocal(out=pgr, in_=sume))
                dv(lambda e: e.tensor_scalar(out=pen, in0=ohg, scalar1=-1.0, scalar2=BIG, op0=ALU.add, op1=ALU.mult))
                dv(lambda e: e.tensor_tensor(out=mm.rearrange("p t (g x) -> p (t g) x", x=EPG),
                                             in0=elt.rearrange("p t (g x) -> p (t g) x", x=EPG),
                                             in1=pen.rearrange("p t g -> p (t g)").unsqueeze(2).to_broadcast([128, T4 * NG, EPG]), op=ALU.add))
                dv(lambda e: e.tensor_reduce(out=m1, in_=mm, axis=AX.X, op=ALU.max))
                dv(lambda e: e.tensor_tensor(out=oh1, in0=mm, in1=b3(m1, NE), op=ALU.is_equal))
                dv(lambda e: e.scalar_tensor_tensor(out=mm2, in0=oh1, scalar=-BIG, in1=mm, op0=ALU.mult, op1=ALU.add))
                dv(lambda e: e.tensor_reduce(out=m2, in_=mm2, axis=AX.X, op=ALU.max))
                dv(lambda e: e.tensor_tensor(out=oh2, in0=mm2, in1=b3(m2, NE), op=ALU.is_equal))
                dv(lambda e: e.tensor_tensor(out=dd, in0=m2, in1=m1, op=ALU.subtract))
                op("act", lambda e: e.activation(out=e2, in_=dd, func=AF.Exp), r=RT, w=RT)
                dv(lambda e: e.tensor_scalar(out=e2, in0=e2, scalar1=1.0, scalar2=None, op0=ALU.add))
                dv(lambda e: e.reciprocal(out=e2, in_=e2))
                dv(lambda e: e.tensor_tensor(out=ga, in0=e2, in1=pgr, op=ALU.mult))
                dv(lambda e: e.tensor_tensor(out=gb, in0=pgr, in1=ga, op=ALU.subtract))
                op("dve", lambda e: e.tensor_copy(out=gates[:, ti0:ti0 + T4, 0], in_=ga), r=RT, w=["gates"])
                op("dve", lambda e: e.tensor_copy(out=gates[:, ti0:ti0 + T4, 1], in_=gb), r=RT, w=["gates"])
                dv(lambda e: e.tensor_tensor(out=tmpe, in0=oh1, in1=oh2, op=ALU.add))
                op("dve", lambda e: e.tensor_copy(out=A_all[:, ti0:ti0 + T4, :], in_=tmpe), r=RT, w=[f"Aq{q}"])
                psR, prR = bank()
                for tt in range(TPQ):
                    for pt in range(tt):
                        op("pe", lambda e, pt=pt: e.matmul(psR[:, tt * NE:(tt + 1) * NE], lhsT=onesb[:], rhs=A_all[:, ti0 + pt, :],
                                                           start=(pt == 0), stop=False), r=["onesb", f"Aq{q}"], w=[prR])
                    op("pe", lambda e: e.matmul(psR[:, tt * NE:(tt + 1) * NE], lhsT=triub[:], rhs=A_all[:, ti0 + tt, :],
                                                start=(tt == 0), stop=True), r=["triub", f"Aq{q}"], w=[prR])
                psC, prC = bank()
                for tt in range(TPQ):
                    op("pe", lambda e: e.matmul(psC[:, :NE], lhsT=onesb[:], rhs=A_all[:, ti0 + tt, :], start=(tt == 0), stop=(tt == TPQ - 1)),
                       r=["onesb", f"Aq{q}"], w=[prC])
                dv(lambda e: e.tensor_tensor(out=slf, in0=psR[:, :T4 * NE].rearrange("p (t n) -> p t n", n=NE),
                                             in1=ecst[:, :].unsqueeze(1).to_broadcast([128, T4, NE]), op=ALU.add), [prR, "ecst"])
                dv(lambda e: e.tensor_tensor(out=slf, in0=slf, in1=cntq[:, :].unsqueeze(1).to_broadcast([128, T4, NE]), op=ALU.add), ["cntq"])
                op("dve", lambda e: e.tensor_tensor(out=cntq[:, :], in0=cntq[:, :], in1=psC[:, :NE], op=ALU.add), r=RT + [prC, "cntq"], w=["cntq"])
                for kk, (oh, sf) in enumerate(((oh1, s0f), (oh2, s1f))):
                    dv(lambda e, oh=oh: e.tensor_tensor(out=tmpe, in0=slf, in1=oh, op=ALU.mult))
                    dv(lambda e, sf=sf: e.tensor_reduce(out=sf, in_=tmpe, axis=AX.X, op=ALU.add))
                    op("dve", lambda e, kk=kk, sf=sf: e.tensor_copy(out=slots_i[:, ti0:ti0 + T4, kk], in_=sf), r=RT, w=["slots_i"])
                si8 = slots_i[:, ti0:ti0 + T4, :].rearrange("p t k -> p (t k)")
                fiq = smi8[q % 2]
                ai8, bi8, fi8 = fiq[:, 0:2 * T4], fiq[:, 2 * T4:4 * T4], fiq[:, 4 * T4:6 * T4]
                FR = [f"smi8_{q % 2}"]
                op("dve", lambda e: e.tensor_scalar(out=ai8, in0=si8, scalar1=127, scalar2=LNEB, op0=ALU.bitwise_and,
                                                    op1=ALU.logical_shift_left), r=["slots_i"] + FR, w=FR)
                op("dve", lambda e: e.tensor_scalar(out=bi8, in0=si8, scalar1=7, scalar2=None, op0=ALU.logical_shift_right),
                   r=["slots_i"] + FR, w=FR)
                op("dve", lambda e: e.tensor_tensor(out=fi8, in0=ai8, in1=bi8, op=ALU.bitwise_or), r=FR, w=FR)
                for tt in range(TPQ):
                    for kk in range(2):
                        c = 2 * tt + kk
                        op("gq", lambda e, c=c, tt=tt: e.indirect_dma_start(out=tki_d, out_offset=bass.IndirectOffsetOnAxis(ap=fi8[:, c:c + 1], axis=0),
                                                                            in_=tokid[:, ti0 + tt:ti0 + tt + 1], in_offset=None),
                           r=FR + ["tokid", "tki_d"], w=[f"tki_{ti0 + tt}_{kk}"])
            pump_ada(99)
            T.barrier()

        op("sq", lambda e: e.dma_start(out=tokidx[:], in_=tki_d.rearrange("(p n) o -> p (n o)", p=128)), r=["tki_d"], w=["tokidx"])
        with contextlib.ExitStack() as s4:
            NWE = 3
            wgu = [sb(f"wgu{j}", [128, KD, 2 * DE], BF16, s4) for j in range(NWE)]
            wdn = [sb(f"wdn{j}", [128, KE, D], BF16, s4) for j in range(2)]
            hgT = [sb(f"hgT{j}", [128, KD, 128], BF16, s4) for j in range(2)]
            sg = [sb(f"sg{j}", [128, DEW], F32, s4) for j in range(2)]
            actb = [sb(f"actb{j}", [128, DE], BF16, s4) for j in range(2)]
            actT = [sb(f"actT{j}", [128, KE, 128], BF16, s4) for j in range(2)]
            ysb = [sb(f"ysb{j}", [128, D], BF16, s4) for j in range(2)]

            NHG = 4
            hg = [sb(f"hgr{j}", [128, D], BF16, s4) for j in range(NHG)]
            NBLK = NE * B
            psg, prg = psb[0], "ps0"
            psu, pru = psb[1], "ps1"
            dnb = [(psb[2], "ps2"), (psb[3], "ps3"), (psb[4], "ps4")]
            psT2 = psb[5][:].bitcast(BF16)
            dn_rr = [0]

            def load_gu(e_):
                j = e_ % NWE
                op("gq", lambda e: e.dma_start(out=wgu[j][:], in_=wgu_d[e_]), w=[f"wgu{j}"])

            def load_dn(e_):
                j = e_ % 2
                op("gq", lambda e: e.dma_start(out=wdn[j][:], in_=wdn_d[e_]), w=[f"wdn{j}"])

            def gather(i):
                jh = i % NHG
                op("gq", lambda e: e.indirect_dma_start(out=hg[jh][:, :], out_offset=None, in_=h2_d,
                                                        in_offset=bass.IndirectOffsetOnAxis(ap=tokidx[:, i:i + 1], axis=0),
                                                        bounds_check=BC[0], oob_is_err=False),
                   r=["tokidx", "h2_d"], w=[f"hgr{jh}"])

            def S1(i):
                jh, j = i % NHG, i % 2
                for k in range(KD):
                    op("pe", lambda e, k=k: e.transpose(out=psT[:, k * 128:(k + 1) * 128], in_=hg[jh][:, k * 128:(k + 1) * 128],
                                                        identity=identb[:]), r=[f"hgr{jh}", "identb"], w=["psT"])
                hk = KD // 2
                op("act", lambda e: e.copy(out=hgT[j][:, :hk, :].rearrange("p k t -> p (k t)"), in_=psT[:, :hk * 128]),
                   r=["psT"], w=[f"hgT{j}a"])
                op("dve", lambda e: e.tensor_copy(out=hgT[j][:, hk:, :].rearrange("p k t -> p (k t)"), in_=psT[:, hk * 128:KD * 128]),
                   r=["psT"], w=[f"hgT{j}b"])
                if i + NHG < NBLK:
                    gather(i + NHG)

            def S2(i):
                j, je = i % 2, (i // B) % NWE
                for dg in range(NDG):
                    for (pp_, pr_, c0) in ((psg, prg, dg * DEW), (psu, pru, DE + dg * DEW)):
                        for k in range(KD):
                            op("pe", lambda e, k=k, pp_=pp_, c0=c0: e.matmul(pp_[:, :DEW], lhsT=hgT[j][:, k, :], rhs=wgu[je][:, k, c0:c0 + DEW],
                                                                              start=(k == 0), stop=(k == KD - 1)),
                               r=[f"hgT{j}a", f"hgT{j}b", f"wgu{je}"], w=[pr_])
                    op("act", lambda e: e.activation(out=sg[j][:], in_=psg[:, :DEW], func=AF.Silu), r=[prg], w=[f"sg{j}"])
                    op("dve", lambda e: e.tensor_tensor(out=actb[j][:, dg * DEW:(dg + 1) * DEW], in0=sg[j][:], in1=psu[:, :DEW], op=ALU.mult),
                       r=[f"sg{j}", pru], w=[f"actb{j}"])

            def S3a(i):
                j = i % 2
                for k in range(KE):
                    op("pe", lambda e, k=k: e.transpose(out=psT2[:, k * 128:(k + 1) * 128], in_=actb[j][:, k * 128:(k + 1) * 128],
                                                        identity=identb[:]), r=[f"actb{j}", "identb"], w=["ps5"])
                op("dve", lambda e: e.tensor_copy(out=actT[j][:].rearrange("p k t -> p (k t)"), in_=psT2[:, :KE * 128]),
                   r=["ps5"], w=[f"actT{j}"])

            def S3b(i):
                j, je = i % 2, (i // B) % 2
                for og in range(NOG):
                    ps, pr = dnb[dn_rr[0]]
                    dn_rr[0] = (dn_rr[0] + 1) % 3
                    for k in range(KE):
                        op("pe", lambda e, k=k: e.matmul(ps[:, :OGW], lhsT=actT[j][:, k, :], rhs=wdn[je][:, k, og * OGW:(og + 1) * OGW],
                                                         start=(k == 0), stop=(k == KE - 1)), r=[f"actT{j}", f"wdn{je}"], w=[pr])
                    if og % 2 == 0:
                        op("act", lambda e: e.copy(out=ysb[j][:, og * OGW:(og + 1) * OGW], in_=ps[:, :OGW]), r=[pr], w=[f"ysb{j}"])
                    else:
                        op("dve", lambda e: e.tensor_copy(out=ysb[j][:, og * OGW:(og + 1) * OGW], in_=ps[:, :OGW]), r=[pr], w=[f"ysb{j}"])
                op("sq", lambda e: e.dma_start(out=y_d[i * 128:(i + 1) * 128, :], in_=ysb[j][:]), r=[f"ysb{j}"], w=["y_d"])

            _bcr = nc.gpsimd.alloc_register("bcreg")
            nc.gpsimd.reg_mov(_bcr, S - 1)
            BC = [nc.gpsimd.snap(_bcr)]
            for jh in range(NHG):
                op("dve", lambda e, jh=jh: e.memset(hg[jh][:], 0.0), w=[f"hgr{jh}"])
            load_gu(0)
            load_dn(0)
            if NE > 1:
                load_gu(1)
            for i in range(min(NHG, NBLK)):
                gather(i)
            S1(0)
            for i in range(NBLK):
                if i >= 1:
                    S3a(i - 1)
                if i + 1 < NBLK:
                    S1(i + 1)
                S2(i)
                if i >= 1:
                    S3b(i - 1)
                if i % B == 0:
                    if i // B + 1 < NE:
                        load_dn(i // B + 1)
                    if i // B + 2 < NE:
                        load_gu(i // B + 2)
            S3a(NBLK - 1)
            S3b(NBLK - 1)
            T.barrier()

        with contextlib.ExitStack() as s5:
            ya = [sb(f"ya{j}", [128, D], BF16, s5) for j in range(2)]
            yb = [sb(f"yb{j}", [128, D], BF16, s5) for j in range(2)]
            x1t = [sb(f"x1t{j}", [128, D], F32, s5) for j in range(2)]
            ft = [sb(f"ft{j}", [128, D], F32, s5) for j in range(2)]
            junk = sb("junk", [128, D], BF16, s5)
            gate2b = sb("gate2b", [128, D], F32, s5)
            gvfb = sb("gvfb", [128, D], F32, s5)
            shfb = sb("shfb", [128, D], F32, s5)
            for v, (t_, r_) in enumerate(((gate2b, "gate2b"), (shfb, "shfb"), (gvfb, "gvfb"))):
                op("sq", lambda e, v=v, t_=t_: e.dma_start(out=t_[:], in_=modv_d[v]), r=["modv_d"], w=[r_])
            def FX(ti):
                j = ti % 2
                op("gq", lambda e: e.indirect_dma_start(out=ya[j][:, :], out_offset=None, in_=y_d,
                                                        in_offset=bass.IndirectOffsetOnAxis(ap=slots_i[:, ti, 0:1], axis=0)),
                   r=["slots_i", "y_d"], w=[f"ya{j}"])
                op("gq", lambda e: e.indirect_dma_start(out=yb[j][:, :], out_offset=None, in_=y_d,
                                                        in_offset=bass.IndirectOffsetOnAxis(ap=slots_i[:, ti, 1:2], axis=0)),
                   r=["slots_i", "y_d"], w=[f"yb{j}"])
                op("sq", lambda e: e.dma_start(out=x1t[j][:], in_=x1_d[ti * 128:(ti + 1) * 128, :]), r=["x1_d"], w=[f"x1t{j}"])
                F = [f"ft{j}"]
                op("act", lambda e: e.activation(out=ft[j][:], in_=ya[j][:], func=AF.Identity, scale=gates[:, ti, 0:1]),
                   r=[f"ya{j}", "gates"], w=F)
                op("dve", lambda e: e.scalar_tensor_tensor(out=ft[j][:], in0=yb[j][:], scalar=gates[:, ti, 1:2], in1=ft[j][:],
                                                           op0=ALU.mult, op1=ALU.add), r=[f"yb{j}", "gates"] + F, w=F)
                op("dve", lambda e: e.tensor_tensor(out=ft[j][:], in0=ft[j][:], in1=gate2b[:], op=ALU.mult), r=F + ["gate2b"], w=F)
                op("dve", lambda e: e.tensor_tensor(out=ft[j][:], in0=ft[j][:], in1=x1t[j][:], op=ALU.add), r=F + [f"x1t{j}"], w=F)
                ssF = sm[:, 4 + 2 * j:5 + 2 * j]
                op("dve", lambda e: e.memset(ssF, 0.0), w=[f"ssF{j}"])
                op("act", lambda e: e.activation(out=junk[:], in_=ft[j][:], func=AF.Square, accum_out=ssF), r=F + [f"ssF{j}"],
                   w=["junk", f"ssF{j}"])

            def FY(ti):
                j = ti % 2
                F = [f"ft{j}"]
                ssF, rsF = sm[:, 4 + 2 * j:5 + 2 * j], sm[:, 5 + 2 * j:6 + 2 * j]
                op("dve", lambda e: e.scalar_tensor_tensor(out=ft[j][:], in0=ft[j][:], scalar=rsF, in1=gvfb[:],
                                                           op0=ALU.mult, op1=ALU.mult), r=F + [f"rsF{j}", "gvfb"], w=F)
                op("dve", lambda e: e.tensor_tensor(out=ft[j][:], in0=ft[j][:], in1=shfb[:], op=ALU.add), r=F + ["shfb"], w=F)
                op("sq", lambda e: e.dma_start(out=out_d[ti * 128:(ti + 1) * 128, :], in_=ft[j][:]), r=F, w=["out_d"])

            FX(0)
            for ti in range(NT):
                j = ti % 2
                rstd_from_ss(sm[:, 4 + 2 * j:5 + 2 * j], sm[:, 5 + 2 * j:6 + 2 * j], D, [f"ssF{j}"], [f"rsF{j}"])
                if ti + 1 < NT:
                    FX(ti + 1)
                FY(ti)
            T.barrier()
    return nc


def _tile_w(w, cw):
    K_, N_ = w.shape
    return np.ascontiguousarray(w.reshape(K_ // 128, 128, N_ // cw, cw).transpose(2, 1, 0, 3), dtype=np.float32)


def host_shared(cfg, w_ada, b_ada, norm1_g, w_in, w_out, gmlp_w_s, gmlp_b_s, gmlp_v_gain, conv_w, conv_b, norm2_g,
                w_router_group, b_router_group, w_router_expert, b_router_expert, w_gate_up, w_down, w_ada_final,
                b_ada_final, norm_f_g, **_):
    D, S, BW, C = cfg["D"], cfg["S"], cfg["BW"], cfg["C"]
    NE = cfg["NG"] * cfg["EPG"]
    KD, NT, BWC = D // 128, S // 128, BW // 128
    CWA, OGW = min(512, D), min(512, D)
    f = lambda a: np.ascontiguousarray(a, dtype=np.float32)
    return {
        "w_ada": _tile_w(w_ada[0], CWA), "b_ada": f(b_ada[0]), "norm1_g": f(norm1_g[0]),
        "w_in": _tile_w(w_in[0], 512), "w_out": _tile_w(w_out[0], OGW),
        "wsT": f(gmlp_w_s[0].transpose(2, 0, 1).reshape(128, -1)),
        "b_s": f(gmlp_b_s[0].reshape(-1)), "v_gain": f(gmlp_v_gain[0].reshape(-1)),
        "conv_w_pk": f(conv_w[0].reshape(3, BWC, 128).transpose(2, 0, 1).reshape(128, -1)),
        "conv_b_pk": f(conv_b[0].reshape(BWC, 128).T),
        "norm2_g": f(norm2_g[0]),
        "wr": f(np.concatenate([w_router_group[0], w_router_expert[0]], axis=1)),
        "rb": f(np.concatenate([b_router_group[0], b_router_expert[0]])),
        "w_gate_up": f(w_gate_up[0].reshape(NE, KD, 128, -1).transpose(0, 2, 1, 3)),
        "w_down": f(w_down[0].reshape(NE, -1, 128, D).transpose(0, 2, 1, 3)),
        "w_ada_final": _tile_w(w_ada_final, CWA), "b_ada_final": f(b_ada_final), "norm_f_g": f(norm_f_g),
        "ident": np.eye(128, dtype=np.float32),
        "triu": np.triu(np.ones((128, 128), np.float32), 1),
        "triu0": np.triu(np.ones((128, 128), np.float32), 0),
        "ecst": np.tile((np.arange(NE) * C).astype(np.float32)[None, :], (128, 1)),
        "tokid": np.ascontiguousarray((np.arange(NT)[None, :] * 128 + np.arange(128)[:, None]).astype(np.int32)),
    }


def host_inputs(cfg, b, shared=None, **inputs):
    if shared is None:
        shared = host_shared(cfg, **inputs)
    KD = cfg["D"] // 128
    m = dict(shared)
    m["x"] = np.ascontiguousarray(inputs["x"][b], dtype=np.float32)
    m["c_pk"] = np.ascontiguousarray(np.asarray(inputs["c"][b], dtype=np.float32).reshape(KD, 128).T)
    return m


_NC_CACHE = {}


def kernel(**inputs):
    cfg = REAL
    inputs = {k: np.asarray(v) for k, v in inputs.items()}
    nb = inputs["x"].shape[0]
    if "nc" not in _NC_CACHE:
        _NC_CACHE["nc"] = build(cfg)
    nc = _NC_CACHE["nc"]
    shared = host_shared(cfg, **inputs)
    in_maps = [host_inputs(cfg, b, shared=shared, **inputs) for b in range(nb)]
    res = run_bass_kernel_spmd(nc, in_maps, core_ids=list(range(nb)))
    return np.stack([np.asarray(r["out"], dtype=np.float32) for r in res.results], axis=0)
```

```python
import contextlib
import numpy as np
import concourse.bass as bass
import concourse.mybir as mybir
from concourse.bass_utils import run_bass_kernel_spmd

F32 = mybir.dt.float32
BF16 = mybir.dt.bfloat16
I32 = mybir.dt.int32
AF = mybir.ActivationFunctionType
ALU = mybir.AluOpType
AX = mybir.AxisListType
EPS = 1e-6
BIG = 30000.0

REAL = dict(D=2048, S=2048, AW=1024, BW=1024, NG=4, EPG=8, DE=512, C=512, TN=512)


class _Stream:
    def __init__(self, name, eng, sems, inc):
        self.name, self.eng, self.sems, self.inc = name, eng, sems, inc
        self.cnt = [0] * len(sems)
        self.rr = 0


class _Eng:
    def __init__(self, name, h):
        self.name, self.h, self.seen = name, h, {}


class Trk:
    def __init__(self, nc, es, ndma=8):
        mk = lambda n: es.enter_context(nc.semaphore(n))
        self.E = {n: _Eng(n, h) for n, h in [("pe", nc.tensor), ("act", nc.scalar), ("dve", nc.vector),
                                             ("pool", nc.gpsimd), ("sp", nc.sync)]}
        self.S = {}
        for n in ["pe", "act", "dve", "pool"]:
            self.S[n] = _Stream(n, self.E[n], [mk("c_" + n)], 1)
        self.S["sq"] = _Stream("sq", self.E["sp"], [mk(f"sq{i}") for i in range(ndma)], 16)
        self.S["gq"] = _Stream("gq", self.E["pool"], [mk(f"gq{i}") for i in range(ndma)], 16)
        self.lastw = {}
        self.rds = {}
        self.nwait = 0

    def _wait(self, eng, tok):
        st, idx, val = tok
        key = (st.name, idx)
        if eng.seen.get(key, 0) >= val:
            return
        eng.h.wait_ge(st.sems[idx], val)
        eng.seen[key] = val
        self.nwait += 1

    def op(self, sname, fn, r=(), w=()):
        st = self.S[sname]
        eng = st.eng
        deps = []
        for reg in r:
            t = self.lastw.get(reg)
            if t:
                deps.append(t)
        for reg in w:
            t = self.lastw.get(reg)
            if t:
                deps.append(t)
            for (sn, idx), val in self.rds.get(reg, {}).items():
                deps.append((self.S[sn], idx, val))
        if st.inc == 16:
            idx = st.rr
            st.rr = (st.rr + 1) % len(st.sems)
            if st.cnt[idx] > 0:
                deps.append((st, idx, st.cnt[idx]))
        else:
            idx = 0
        for t in deps:
            if t[0] is st and sname == "pe":
                continue
            self._wait(eng, t)
        ins = fn(eng.h)
        st.cnt[idx] += st.inc
        ins.then_inc(st.sems[idx], st.inc)
        tok = (st, idx, st.cnt[idx])
        for reg in w:
            self.lastw[reg] = tok
            self.rds[reg] = {}
        for reg in r:
            if reg not in w:
                self.rds.setdefault(reg, {})[(sname, idx)] = tok[2]
        return tok

    def barrier(self):
        for eng in self.E.values():
            for st in self.S.values():
                for idx, c in enumerate(st.cnt):
                    if c > 0:
                        self._wait(eng, (st, idx, c))
        self.lastw = {}
        self.rds = {}


def build(cfg, gelu_mode="tanh"):
    D, S, AW, BW = cfg["D"], cfg["S"], cfg["AW"], cfg["BW"]
    NG, EPG, DE, C, TN = cfg["NG"], cfg["EPG"], cfg["DE"], cfg["C"], cfg["TN"]
    H = AW // 128
    NE = NG * EPG
    NR = NG + NE
    KD = D // 128
    NT = S // 128
    TPQ = TN // 128
    NQ = S // TN
    KM = (AW + BW) // 128
    ICOLS = 2 * AW + 3 * BW
    BWC = BW // 128
    OGW = min(512, D)
    NOG = D // OGW
    VW = min(512, AW)
    NVG = AW // VW
    HPG = VW // 128
    CGW = min(512, BW)
    NCG = BW // CGW
    CPG = CGW // 128
    B = C // 128
    NEB = NE * B
    NSLOT = NE * C
    DEW = min(512, DE)
    NDG = DE // DEW
    KE = DE // 128
    KMAX = max(KD, KM)
    CWA = min(512, D)
    NCA = D // CWA
    KPC = CWA // 128
    assert VW == 512 and CGW == 512

    nc = bass.Bass("TRN2", target_bir_lowering=False)

    def din(name, shape, dt=F32):
        return nc.dram_tensor(name, list(shape), dt, kind="ExternalInput").ap()

    x_d = din("x", [S, D])
    c_d = din("c_pk", [128, KD])
    w_ada_d = din("w_ada", [6 * NCA, 128, KD, CWA])
    b_ada_d = din("b_ada", [6 * D])
    n1g_d = din("norm1_g", [D])
    w_in_d = din("w_in", [ICOLS // 512, 128, KD, 512])
    w_out_d = din("w_out", [NOG, 128, KM, OGW])
    wsT_d = din("wsT", [128, H * 128])
    bs_d = din("b_s", [H * 128])
    vg_d = din("v_gain", [AW])
    cw_d = din("conv_w_pk", [128, 3 * BWC])
    cbias_d = din("conv_b_pk", [128, BWC])
    n2g_d = din("norm2_g", [D])
    wr_d = din("wr", [D, NR])
    rb_d = din("rb", [NR])
    wgu_d = din("w_gate_up", [NE, 128, KD, 2 * DE])
    wdn_d = din("w_down", [NE, 128, KE, D])
    w_adaf_d = din("w_ada_final", [2 * NCA, 128, KD, CWA])
    b_adaf_d = din("b_ada_final", [2 * D])
    nfg_d = din("norm_f_g", [D])
    ident_d = din("ident", [128, 128])
    triu_d = din("triu", [128, 128])
    triu0_d = din("triu0", [128, 128])
    ecst_d = din("ecst", [128, NE])
    tokid_d = din("tokid", [128, NT], I32)
    out_d = nc.dram_tensor("out", [S, D], F32, kind="ExternalOutput").ap()
    x1_d = nc.dram_tensor("x1_scr", [S, D], F32, kind="Internal").ap()
    h2_d = nc.dram_tensor("h2_scr", [S, D], BF16, kind="Internal").ap()
    y_d = nc.dram_tensor("y_scr", [NSLOT, D], BF16, kind="Internal").ap()
    tki_d = nc.dram_tensor("tki_scr", [128 * NEB, 1], I32, kind="Internal").ap()
    modv_d = nc.dram_tensor("modv_scr", [3, 128, D], F32, kind="Internal").ap()

    es = contextlib.ExitStack()
    with es:
        T = Trk(nc, es)
        _uid = [0]

        def sb(name, shape, dt=F32, stack=es):
            _uid[0] += 1
            return stack.enter_context(nc.sbuf_tensor(f"{name}_{_uid[0]}", list(shape), dt))
        identf = sb("identf", [128, 128])
        identb = sb("identb", [128, 128], BF16)
        triub = sb("triub", [128, 128], BF16)
        onesb = sb("onesb", [128, 128], BF16)
        ecst = sb("ecst_t", [128, NE])
        tokid = sb("tokid_t", [128, NT], I32)
        wmT = sb("wmT", [128, H, 128], BF16)
        bsb = sb("bsb", [128, H, 128])
        gainb = sb("gainb", [128, AW])
        cw = sb("cw", [128, 3, BWC])
        cbias = sb("cbias", [128, BWC])
        wrb = sb("wrb", [128, KD, NR], BF16)
        rbb = sb("rbb", [128, NR])
        cact = sb("cact", [128, KD])
        cb = sb("cb", [128, KD, 128], BF16)
        gv1p = sb("gv1p", [128, KD])
        sh1p = sb("sh1p", [128, KD])
        gv2p = sb("gv2p", [128, KD])
        sh2p = sb("sh2p", [128, KD])
        A_all = sb("A_all", [128, NT, NE], BF16)
        slots_i = sb("slots_i", [128, NT, 2], I32)
        gates = sb("gates", [128, NT, 2])
        tokidx = sb("tokidx", [128, NEB], I32)
        carry = sb("carry", [128, BWC, 2])
        zt = sb("zt", [128, NEB], I32)
        sm = sb("sm", [128, 64])
        smi = sb("smi", [128, 16], I32)
        LNEB = NEB.bit_length() - 1
        assert (1 << LNEB) == NEB
        psT = es.enter_context(nc.psum_tensor("psT", [128, max(KD * 128, KE * 128, 1024)], BF16))
        NB = 6
        psb = [es.enter_context(nc.psum_tensor(f"ps{i}", [128, 512], F32)) for i in range(NB)]
        bank_rr = [0]

        def bank():
            i = bank_rr[0]
            bank_rr[0] = (i + 1) % NB
            return psb[i], f"ps{i}"

        op = T.op

        op("sq", lambda e: e.dma_start(out=identf[:], in_=ident_d), w=["identf"])
        op("gq", lambda e: e.dma_start(out=identb[:], in_=ident_d), w=["identb"])
        op("gq", lambda e: e.dma_start(out=triub[:], in_=triu_d), w=["triub"])
        op("sq", lambda e: e.dma_start(out=ecst[:], in_=ecst_d), w=["ecst"])
        op("sq", lambda e: e.dma_start(out=tokid[:], in_=tokid_d), w=["tokid"])
        with contextlib.ExitStack() as s0:
            wsf = sb("wsf", [128, H, 128], F32, s0)
            mk0 = sb("mk0", [128, 128], F32, s0)
            op("sq", lambda e: e.dma_start(out=wsf[:].rearrange("p h t -> p (h t)"), in_=wsT_d), w=["wsf"])
            op("sq", lambda e: e.dma_start(out=mk0[:], in_=triu0_d), w=["mk0"])
            op("dve", lambda e: e.tensor_tensor(out=wmT[:], in0=wsf[:], in1=mk0[:].unsqueeze(1).to_broadcast([128, H, 128]), op=ALU.mult),
               r=["wsf", "mk0"], w=["wmT"])
            T.barrier()
        op("sq", lambda e: e.dma_start(out=bsb[:].rearrange("p h t -> p (h t)"), in_=bs_d.partition_broadcast(128)), w=["bsb"])
        op("sq", lambda e: e.dma_start(out=gainb[:], in_=vg_d.partition_broadcast(128)), w=["gainb"])
        op("sq", lambda e: e.dma_start(out=cw[:].rearrange("p a b -> p (a b)"), in_=cw_d), w=["cw"])
        op("sq", lambda e: e.dma_start(out=cbias[:], in_=cbias_d), w=["cbias"])
        op("gq", lambda e: e.dma_start(out=wrb[:], in_=wr_d.rearrange("(k p) n -> p k n", p=128)), w=["wrb"])
        op("sq", lambda e: e.dma_start(out=rbb[:], in_=rb_d.partition_broadcast(128)), w=["rbb"])
        op("sq", lambda e: e.dma_start(out=cact[:], in_=c_d), w=["cact"])
        op("dve", lambda e: e.memset(onesb[:], 1.0), w=["onesb"])
        op("dve", lambda e: e.memset(carry[:], 0.0), w=["carry"])
        op("dve", lambda e: e.memset(zt[:], S), w=["zt"])
        op("sq", lambda e: e.dma_start(out=tki_d.rearrange("(p n) o -> p (n o)", p=128), in_=zt[:]), r=["zt"], w=["tki_d"])
        op("act", lambda e: e.activation(out=cact[:], in_=cact[:], func=AF.Silu), r=["cact"], w=["cact"])
        for k in range(KD):
            op("dve", lambda e, k=k: e.tensor_scalar(out=cb[:, k, :], in0=onesb[:], scalar1=cact[:, k:k + 1], scalar2=None,
                                                     op0=ALU.mult), r=["cact", "onesb"], w=["cb"])

        def rstd_from_ss(ss_ap, rs_ap, n, regs_r, regs_w):
            op("dve", lambda e: e.tensor_scalar(out=rs_ap, in0=ss_ap, scalar1=1.0 / n, scalar2=EPS, op0=ALU.mult, op1=ALU.add),
               r=regs_r, w=regs_w)
            op("act", lambda e: e.activation(out=rs_ap, in_=rs_ap, func=AF.Sqrt), r=regs_w, w=regs_w)
            op("dve", lambda e: e.reciprocal(out=rs_ap, in_=rs_ap), r=regs_w, w=regs_w)

        def gelu(out_ap, in_ap, tmp_ap, r, w, wt):
            if gelu_mode == "tanh":
                op("act", lambda e: e.activation(out=out_ap, in_=in_ap, func=AF.Gelu_apprx_tanh), r=r, w=w)
            else:
                op("act", lambda e: e.activation(out=tmp_ap, in_=in_ap, func=AF.Square), r=r, w=wt)
                op("dve", lambda e: e.tensor_scalar(out=tmp_ap, in0=tmp_ap, scalar1=0.044715 * 1.5957691216, scalar2=1.5957691216,
                                                    op0=ALU.mult, op1=ALU.add), r=wt, w=wt)
                op("dve", lambda e: e.tensor_tensor(out=tmp_ap, in0=tmp_ap, in1=in_ap, op=ALU.mult), r=list(wt) + list(r), w=wt)
                op("act", lambda e: e.activation(out=tmp_ap, in_=tmp_ap, func=AF.Sigmoid), r=wt, w=wt)
                op("dve", lambda e: e.tensor_tensor(out=out_ap, in0=tmp_ap, in1=in_ap, op=ALU.mult), r=list(wt) + list(r), w=w)

        def transpose_mod(src_ap, src_reg, sc_pp, sc_reg, bi_pp, bi_reg, dst_ap, dst_reg):
            for k in range(KD):
                op("pe", lambda e, k=k: e.transpose(out=psT[:, k * 128:(k + 1) * 128], in_=src_ap[:, k * 128:(k + 1) * 128],
                                                    identity=identb[:]), r=[src_reg, "identb"], w=["psT"])
            mt = modtmp_box[0]
            op("dve", lambda e: e.tensor_tensor(out=mt[:].rearrange("p (k t) -> p k t", t=128),
                                                in0=psT[:, :KD * 128].rearrange("p (k t) -> p k t", t=128),
                                                in1=sc_pp[:, :].unsqueeze(2).to_broadcast([128, KD, 128]), op=ALU.mult),
               r=["psT", sc_reg], w=["modtmp"])
            op("dve", lambda e: e.tensor_tensor(out=dst_ap, in0=mt[:].rearrange("p (k t) -> p k t", t=128),
                                                in1=bi_pp[:, :].unsqueeze(2).to_broadcast([128, KD, 128]), op=ALU.add),
               r=["modtmp", bi_reg], w=[dst_reg])

        modtmp_box = [None]

        def ada_vec(stack_tiles, wsrc, bsrc, col0, out_ap, out_reg):
            wa, bb = stack_tiles
            for ci in range(NCA):
                j = ada_rr[0]
                ada_rr[0] = (j + 1) % 2
                cs = slice(col0 + ci * CWA, col0 + (ci + 1) * CWA)
                op("gq", lambda e: e.dma_start(out=wa[j][:, :, :], in_=wsrc[(col0 // CWA) + ci]), w=[f"wa{j}"])
                op("sq", lambda e: e.dma_start(out=bb[j][:, :], in_=bsrc[cs].partition_broadcast(128)), w=[f"bb{j}"])
                ps, pr = bank()
                for k in range(KD):
                    op("pe", lambda e, k=k: e.matmul(ps[:, :CWA], lhsT=cb[:, k, :], rhs=wa[j][:, k, :], start=(k == 0), stop=(k == KD - 1)),
                       r=["cb", f"wa{j}"], w=[pr])
                op("dve", lambda e: e.tensor_tensor(out=out_ap[:, ci * CWA:(ci + 1) * CWA], in0=ps[:, :CWA], in1=bb[j][:, :], op=ALU.add),
                   r=[pr, f"bb{j}"], w=[out_reg])

        ada_rr = [0]

        def gain_mul(vec_ap, vec_reg, gsrc, tmp_ap, tmp_reg):
            op("sq", lambda e: e.dma_start(out=tmp_ap, in_=gsrc.partition_broadcast(128)), w=[tmp_reg])
            op("dve", lambda e: e.scalar_tensor_tensor(out=vec_ap, in0=vec_ap, scalar=1.0, in1=tmp_ap, op0=ALU.add, op1=ALU.mult),
               r=[vec_reg, tmp_reg], w=[vec_reg])

        def to_pp(vec_ap, vec_reg, pp_ap, pp_reg, tmp_ap, tmp_reg):
            op("dve", lambda e: e.tensor_tensor(out=tmp_ap.rearrange("p (k j) -> p k j", j=128),
                                                in0=vec_ap.rearrange("p (k j) -> p k j", j=128),
                                                in1=identf[:].unsqueeze(1).to_broadcast([128, KD, 128]), op=ALU.mult),
               r=[vec_reg, "identf"], w=[tmp_reg])
            op("dve", lambda e: e.tensor_reduce(out=pp_ap, in_=tmp_ap.rearrange("p (k j) -> p k j", j=128), axis=AX.X, op=ALU.add),
               r=[tmp_reg], w=[pp_reg])

        gate1b = sb("gate1b", [128, D])
        with contextlib.ExitStack() as s1:
            wa = [sb(f"wa{j}", [128, KD, CWA], BF16, s1) for j in range(2)]
            bb = [sb(f"bb{j}", [128, CWA], F32, s1) for j in range(2)]
            va = sb("ada_va", [128, D], F32, s1)
            vb = sb("ada_vb", [128, D], F32, s1)
            ada_vec((wa, bb), w_ada_d, b_ada_d, 0 * D, va[:], "ada_va")
            to_pp(va[:], "ada_va", sh1p[:], "sh1p", vb[:], "ada_vb")
            ada_vec((wa, bb), w_ada_d, b_ada_d, 1 * D, va[:], "ada_va")
            gain_mul(va[:], "ada_va", n1g_d, vb[:], "ada_vb")
            to_pp(va[:], "ada_va", gv1p[:], "gv1p", vb[:], "ada_vb")
            T.barrier()

        with contextlib.ExitStack() as s2:
            NWB = 3
            wbig = [sb(f"wbig{j}", [128, KMAX, 512], BF16, s2) for j in range(NWB)]
            wrr = [0]

            def load_w(src, nk, ncols):
                j = wrr[0]
                wrr[0] = (j + 1) % NWB
                op("gq", lambda e: e.dma_start(out=wbig[j][:, :nk, :ncols], in_=src), w=[f"wbig{j}"])
                return wbig[j], f"wbig{j}"

            h1T = sb("h1T", [128, KD, TN], BF16, s2)
            vn = sb("vn", [128, TPQ, AW], BF16, s2)
            yT = sb("yT", [128, KM, TN], BF16, s2)
            xq = sb("xq", [128, TPQ, D], F32, s2)
            xn = [sb(f"xn{j}", [128, D], BF16, s2) for j in range(2)]
            h2T = sb("h2T", [128, KD, 128], BF16, s2)
            vt = [sb(f"vt{j}", [128, VW], F32, s2) for j in range(2)]
            vsq = [sb(f"vsq{j}", [128, VW], F32, s2) for j in range(2)]
            uT = [sb(f"uT{j}", [128, TN], BF16, s2) for j in range(2)]
            gtmp = [sb(f"gtmp{j}", [128, 512], F32, s2) for j in range(2)] if gelu_mode != "tanh" else None
            gt = lambda j, n: gtmp[j][:, :n] if gtmp is not None else None
            modtmp_box[0] = sb("modtmp", [128, KD * 128], F32, s2)
            _ztmp1 = sb("ztmp0", [128, TN], F32, s2)
            ztmp = [_ztmp1, _ztmp1]
            cT = sb("cT", [128, CPG, TN], F32, s2)
            pre = sb("pre", [128, CPG, TN + 2], F32, s2)
            _cv1 = sb("cv0", [128, TN], F32, s2)
            cv = [_cv1, _cv1]
            _otmp1 = sb("otmp0", [128, OGW], F32, s2)
            otmp = [_otmp1, _otmp1]
            rt4 = [sb(f"rt4_{i}", [128, TPQ, NE], F32, s2) for i in range(10)]
            smr = sb("smr", [128, 32], F32, s2)
            smr2 = sb("smr2", [128, 11 * TPQ], F32, s2)
            smi8 = [sb(f"smi8_{i}", [128, 6 * TPQ], I32, s2) for i in range(2)]
            rtg = [sb(f"rtg_{i}", [128, TPQ, NG], F32, s2) for i in range(4)]
            cntq = sb("cntq", [128, NE], F32, s2)
            op("dve", lambda e: e.memset(cntq[:], 0.0), w=["cntq"])

            ada_q = []
            for kind, wsrc, bsrc, col0, gsrc in (("gate1", w_ada_d, b_ada_d, 2 * D, None), ("sh2", w_ada_d, b_ada_d, 3 * D, None),
                                                 ("gv2", w_ada_d, b_ada_d, 4 * D, n2g_d), ("gate2", w_ada_d, b_ada_d, 5 * D, None),
                                                 ("shf", w_adaf_d, b_adaf_d, 0, None), ("gvf", w_adaf_d, b_adaf_d, D, nfg_d)):
                for ci in range(NCA):
                    ada_q.append((kind, wsrc, bsrc, col0, gsrc, ci))
            N_EARLY = 3 * NCA

            def ada_load(spec):
                kind, wsrc, bsrc, col0, gsrc, ci = spec
                wa_, war = load_w(wsrc[(col0 // CWA) + ci], KD, CWA)
                return spec, wa_, war

            def ada_chunk(spec):
                ada_compute(*ada_load(spec))

            ada_pending = [None]

            def pump_tail(last_in_quarter):
                if ada_pending[0] is not None:
                    ada_compute(*ada_pending[0])
                    ada_pending[0] = None
                if not last_in_quarter and ada_done[0] < len(ada_q):
                    ada_pending[0] = ada_load(ada_q[ada_done[0]])
                    ada_done[0] += 1

            def ada_compute(spec, wa_, war):
                kind, wsrc, bsrc, col0, gsrc, ci = spec
                cs = slice(col0 + ci * CWA, col0 + (ci + 1) * CWA)
                ls = slice(ci * CWA, (ci + 1) * CWA)
                bbm, gbm, adt, adt2 = vt[0][:, :CWA], vt[1][:, :CWA], vsq[0][:, :CWA], vsq[1][:, :CWA]
                op("sq", lambda e: e.dma_start(out=bbm, in_=bsrc[cs].partition_broadcast(128)), w=["vt0"])
                ps, pr = bank()
                for k in range(KD):
                    op("pe", lambda e, k=k: e.matmul(ps[:, :CWA], lhsT=cb[:, k, :], rhs=wa_[:, k, :CWA], start=(k == 0), stop=(k == KD - 1)),
                       r=["cb", war], w=[pr])
                if kind == "gate1":
                    op("dve", lambda e: e.tensor_tensor(out=gate1b[:, ls], in0=ps[:, :CWA], in1=bbm, op=ALU.add), r=[pr, "vt0"], w=["gate1b"])
                    return
                op("dve", lambda e: e.tensor_tensor(out=adt, in0=ps[:, :CWA], in1=bbm, op=ALU.add), r=[pr, "vt0"], w=["vsq0"])
                if gsrc is not None:
                    op("sq", lambda e: e.dma_start(out=gbm, in_=gsrc[ls].partition_broadcast(128)), w=["vt1"])
                    op("dve", lambda e: e.scalar_tensor_tensor(out=adt, in0=adt, scalar=1.0, in1=gbm, op0=ALU.add, op1=ALU.mult),
                       r=["vsq0", "vt1"], w=["vsq0"])
                if kind in ("sh2", "gv2"):
                    pp_t, pp_r = (sh2p, "sh2p") if kind == "sh2" else (gv2p, "gv2p")
                    op("dve", lambda e: e.tensor_tensor(out=adt2.rearrange("p (k j) -> p k j", j=128),
                                                        in0=adt.rearrange("p (k j) -> p k j", j=128),
                                                        in1=identf[:].unsqueeze(1).to_broadcast([128, KPC, 128]), op=ALU.mult),
                       r=["vsq0", "identf"], w=["vsq1"])
                    op("dve", lambda e: e.tensor_reduce(out=pp_t[:, ci * KPC:(ci + 1) * KPC], in_=adt2.rearrange("p (k j) -> p k j", j=128),
                                                        axis=AX.X, op=ALU.add), r=["vsq1"], w=[pp_r])
                else:
                    v = {"gate2": 0, "shf": 1, "gvf": 2}[kind]
                    op("sq", lambda e: e.dma_start(out=modv_d[v, :, ls], in_=adt), r=["vsq0"], w=["modv_d"])

            ada_done = [0]

            def pump_ada(n, upto=None):
                lim = len(ada_q) if upto is None else upto
                while n > 0 and ada_done[0] < lim:
                    ada_chunk(ada_q[ada_done[0]])
                    ada_done[0] += 1
                    n -= 1

            for q in range(NQ):
                for tt in range(TPQ):
                    ti = q * TPQ + tt
                    xr = f"xq{tt}"
                    op("sq", lambda e: e.dma_start(out=xq[:, tt, :], in_=x_d[ti * 128:(ti + 1) * 128, :]), w=[xr])
                    j = ti % 2
                    ssA = sm[:, 0:1]
                    op("dve", lambda e: e.memset(ssA, 0.0), w=["ssA"])
                    op("act", lambda e: e.activation(out=xn[j][:], in_=xq[:, tt, :], func=AF.Square, accum_out=ssA),
                       r=[xr, "ssA"], w=[f"xn{j}", "ssA"])
                    rstd_from_ss(ssA, sm[:, 1:2], D, ["ssA"], ["rsA"])
                    op("dve", lambda e: e.tensor_scalar(out=xn[j][:], in0=xq[:, tt, :], scalar1=sm[:, 1:2], scalar2=None, op0=ALU.mult),
                       r=[xr, "rsA"], w=[f"xn{j}"])
                    transpose_mod(xn[j], f"xn{j}", gv1p, "gv1p", sh1p, "sh1p", h1T[:, :, tt * 128:(tt + 1) * 128], "h1T")
                for vg in range(NVG):
                    wv, wvr = load_w(w_in_d[(AW + vg * VW) // 512], KD, VW)
                    for tt in range(TPQ):
                        ps, pr = bank()
                        for k in range(KD):
                            op("pe", lambda e, k=k: e.matmul(ps[:, :VW], lhsT=h1T[:, k, tt * 128:(tt + 1) * 128], rhs=wv[:, k, :VW],
                                                             start=(k == 0), stop=(k == KD - 1)), r=["h1T", wvr], w=[pr])
                        j = tt % 2
                        gelu(vt[j][:], ps[:, :VW], gt(j, VW), [pr], [f"vt{j}"], [f"gtmp{j}"])
                        op("dve", lambda e: e.tensor_tensor(out=vsq[j][:], in0=vt[j][:], in1=vt[j][:], op=ALU.mult), r=[f"vt{j}"], w=[f"vsq{j}"])
                        vss = smr[:, 0:HPG]
                        op("dve", lambda e: e.tensor_reduce(out=vss, in_=vsq[j][:].rearrange("p (h d) -> p h d", d=128), axis=AX.X, op=ALU.add),
                           r=[f"vsq{j}"], w=["vss"])
                        rstd_from_ss(vss, smr[:, 8:8 + HPG], 128, ["vss"], ["vrs"])
                        op("dve", lambda e: e.tensor_tensor(out=vsq[j][:].rearrange("p (h d) -> p h d", d=128),
                                                            in0=vt[j][:].rearrange("p (h d) -> p h d", d=128),
                                                            in1=smr[:, 8:8 + HPG].unsqueeze(2).to_broadcast([128, HPG, 128]), op=ALU.mult),
                           r=[f"vt{j}", "vrs"], w=[f"vsq{j}"])
                        op("dve", lambda e: e.tensor_tensor(out=vn[:, tt, vg * VW:(vg + 1) * VW], in0=vsq[j][:],
                                                            in1=gainb[:, vg * VW:(vg + 1) * VW], op=ALU.mult),
                           r=[f"vsq{j}", "gainb"], w=["vn"])
                for ug in range(NVG):
                    pump_ada(2, upto=N_EARLY)
                    wu, wur = load_w(w_in_d[(ug * VW) // 512], KD, VW)
                    for hh in range(HPG):
                        h = ug * HPG + hh
                        ps, pr = bank()
                        for k in range(KD):
                            op("pe", lambda e, k=k: e.matmul(ps[:, :TN], lhsT=wu[:, k, hh * 128:(hh + 1) * 128], rhs=h1T[:, k, :],
                                                             start=(k == 0), stop=(k == KD - 1)), r=["h1T", wur], w=[pr])
                        j = h % 2
                        gelu(uT[j][:], ps[:, :TN], gt(j, TN), [pr], [f"uT{j}"], [f"gtmp{j}"])
                        ps2, pr2 = bank()
                        for c in range(TPQ):
                            op("pe", lambda e, c=c: e.matmul(ps2[:, c * 128:(c + 1) * 128], lhsT=vn[:, c, h * 128:(h + 1) * 128],
                                                             rhs=wmT[:, h, :], start=True, stop=True), r=["vn", "wmT"], w=[pr2])
                        op("dve", lambda e: e.tensor_tensor(out=ztmp[j][:].rearrange("p (c t) -> p c t", t=128),
                                                            in0=ps2[:, :TN].rearrange("p (c t) -> p c t", t=128),
                                                            in1=bsb[:, h, :].unsqueeze(1).to_broadcast([128, TPQ, 128]), op=ALU.add),
                           r=[pr2, "bsb"], w=["ztmp0"])
                        op("dve", lambda e: e.tensor_tensor(out=yT[:, h, :], in0=ztmp[j][:], in1=uT[j][:], op=ALU.mult),
                           r=["ztmp0", f"uT{j}"], w=[f"yT{h}"])
                for cg in range(NCG):
                    base = 2 * AW
                    pump_ada(2, upto=N_EARLY)
                    wc, wcr = load_w(w_in_d[(base + BW + cg * CGW) // 512], KD, CGW)
                    for jx in range(CPG):
                        ps, pr = bank()
                        for k in range(KD):
                            op("pe", lambda e, k=k: e.matmul(ps[:, :TN], lhsT=wc[:, k, jx * 128:(jx + 1) * 128], rhs=h1T[:, k, :],
                                                             start=(k == 0), stop=(k == KD - 1)), r=["h1T", wcr], w=[pr])
                        op("act", lambda e: e.copy(out=cT[:, jx, :], in_=ps[:, :TN]), r=[pr], w=[f"cT{jx}"])
                    pump_ada(2, upto=N_EARLY)
                    wx, wxr = load_w(w_in_d[(base + 2 * BW + cg * CGW) // 512], KD, CGW)
                    for jx in range(CPG):
                        jj = cg * CPG + jx
                        ps, pr = bank()
                        for k in range(KD):
                            op("pe", lambda e, k=k: e.matmul(ps[:, :TN], lhsT=wx[:, k, jx * 128:(jx + 1) * 128], rhs=h1T[:, k, :],
                                                             start=(k == 0), stop=(k == KD - 1)), r=["h1T", wxr], w=[pr])
                        op("dve", lambda e: e.tensor_copy(out=pre[:, jx, 0:2], in_=carry[:, jj, :]), r=["carry"], w=[f"pre{jx}"])
                        op("dve", lambda e: e.tensor_tensor(out=pre[:, jx, 2:TN + 2], in0=ps[:, :TN], in1=cT[:, jx, :], op=ALU.mult),
                           r=[pr, f"cT{jx}"], w=[f"pre{jx}"])
                        op("dve", lambda e: e.tensor_copy(out=carry[:, jj, :], in_=pre[:, jx, TN:TN + 2]), r=[f"pre{jx}"], w=["carry"])
                    pump_ada(2, upto=N_EARLY)
                    wb, wbr = load_w(w_in_d[(base + cg * CGW) // 512], KD, CGW)
                    for jx in range(CPG):
                        jj = cg * CPG + jx
                        j = jj % 2
                        ps, pr = bank()
                        for k in range(KD):
                            op("pe", lambda e, k=k: e.matmul(ps[:, :TN], lhsT=wb[:, k, jx * 128:(jx + 1) * 128], rhs=h1T[:, k, :],
                                                             start=(k == 0), stop=(k == KD - 1)), r=["h1T", wbr], w=[pr])
                        op("dve", lambda e: e.tensor_scalar(out=cv[j][:], in0=pre[:, jx, 0:TN], scalar1=cw[:, 0, jj:jj + 1], scalar2=None,
                                                            op0=ALU.mult), r=[f"pre{jx}", "cw"], w=["cv0"])
                        for kk in (1, 2):
                            op("dve", lambda e, kk=kk: e.scalar_tensor_tensor(out=cv[j][:], in0=pre[:, jx, kk:kk + TN],
                                                                              scalar=cw[:, kk, jj:jj + 1], in1=cv[j][:],
                                                                              op0=ALU.mult, op1=ALU.add),
                               r=[f"pre{jx}", "cw", "cv0"], w=["cv0"])
                        op("dve", lambda e: e.scalar_tensor_tensor(out=yT[:, H + jj, :], in0=cv[j][:], scalar=cbias[:, jj:jj + 1],
                                                                   in1=ps[:, :TN], op0=ALU.add, op1=ALU.mult),
                           r=["cv0", "cbias", pr], w=[f"yT{H + jj}"])
                pump_ada(99, upto=N_EARLY)
                yregs = [f"yT{k}" for k in range(KM)]
                for og in range(NOG):
                    wo, wor = load_w(w_out_d[og], KM, OGW)
                    for tt in range(TPQ):
                        ps, pr = bank()
                        for k in range(KM):
                            op("pe", lambda e, k=k: e.matmul(ps[:, :OGW], lhsT=yT[:, k, tt * 128:(tt + 1) * 128], rhs=wo[:, k, :OGW],
                                                             start=(k == 0), stop=(k == KM - 1)), r=[yregs[k], wor], w=[pr])
                        j = tt % 2
                        xr = f"xq{tt}"
                        op("dve", lambda e: e.tensor_tensor(out=otmp[j][:], in0=ps[:, :OGW], in1=gate1b[:, og * OGW:(og + 1) * OGW], op=ALU.mult),
                           r=[pr, "gate1b"], w=["otmp0"])
                        op("dve", lambda e: e.tensor_tensor(out=xq[:, tt, og * OGW:(og + 1) * OGW], in0=otmp[j][:],
                                                            in1=xq[:, tt, og * OGW:(og + 1) * OGW], op=ALU.add),
                           r=["otmp0", xr], w=[xr])
                ti0 = q * TPQ
                T4 = TPQ
                ssq, rsq = smr[:, 0:T4], smr[:, 4:4 + T4]
                RT = ["rt"]
                op("dve", lambda e: e.memset(ssq, 0.0), w=["ssq"])
                for tt in range(TPQ):
                    ti = ti0 + tt
                    xr = f"xq{tt}"
                    pump_tail(tt == TPQ - 1)
                    op("sq", lambda e: e.dma_start(out=x1_d[ti * 128:(ti + 1) * 128, :], in_=xq[:, tt, :]), r=[xr], w=["x1_d"])
                    op("act", lambda e: e.activation(out=xn[tt % 2][:], in_=xq[:, tt, :], func=AF.Square, accum_out=ssq[:, tt:tt + 1]),
                       r=[xr, "ssq"], w=[f"xn{tt % 2}", "ssq"])
                rstd_from_ss(ssq, rsq, D, ["ssq"], ["rsq"])
                psL, prL = bank()
                for tt in range(TPQ):
                    ti = ti0 + tt
                    xr = f"xq{tt}"
                    j = tt % 2
                    op("dve", lambda e: e.tensor_scalar(out=xn[j][:], in0=xq[:, tt, :], scalar1=rsq[:, tt:tt + 1], scalar2=None, op0=ALU.mult),
                       r=[xr, "rsq"], w=[f"xn{j}"])
                    transpose_mod(xn[j], f"xn{j}", gv2p, "gv2p", sh2p, "sh2p", h2T[:, :, :], "h2T")
                    for k in range(KD):
                        op("pe", lambda e, k=k: e.matmul(psL[:, tt * NR:(tt + 1) * NR], lhsT=h2T[:, k, :], rhs=wrb[:, k, :],
                                                         start=(k == 0), stop=(k == KD - 1)), r=["h2T", "wrb"], w=[prL])
                    for k in range(KD):
                        op("pe", lambda e, k=k: e.transpose(out=psT[:, k * 128:(k + 1) * 128], in_=h2T[:, k, :], identity=identb[:]),
                           r=["h2T", "identb"], w=["psT"])
                    op("act", lambda e: e.copy(out=xn[j][:], in_=psT[:, :KD * 128]), r=["psT"], w=[f"xn{j}"])
                    op("sq", lambda e: e.dma_start(out=h2_d[ti * 128:(ti + 1) * 128, :], in_=xn[j][:]), r=[f"xn{j}"], w=["h2_d"])
                dv = lambda f, r_=(): op("dve", f, r=RT + list(r_), w=RT)
                glt, elt = rtg[0][:, :, :], rt4[1][:, :, :]
                psL3 = psL[:, :T4 * NR].rearrange("p (t n) -> p t n", n=NR)
                rbb3 = rbb[:, :].unsqueeze(1).to_broadcast([128, T4, NR])
                dv(lambda e: e.tensor_tensor(out=glt, in0=psL3[:, :, 0:NG], in1=rbb3[:, :, 0:NG], op=ALU.add), [prL, "rbb"])
                dv(lambda e: e.tensor_tensor(out=elt, in0=psL3[:, :, NG:NR], in1=rbb3[:, :, NG:NR], op=ALU.add), [prL, "rbb"])
                gmax, sume, pgr, m1, m2, dd, e2, ga, gb, s0f, s1f = (smr2[:, i * T4:(i + 1) * T4] for i in range(11))
                ohg, pen, eg = rtg[1][:, :, :], rtg[2][:, :, :], rtg[3][:, :, :]
                mm, oh1, mm2, oh2, slf, tmpe = rt4[5][:, :, :], rt4[6][:, :, :], rt4[7][:, :, :], rt4[8][:, :, :], rt4[9][:, :, :], rt4[2][:, :, :]
                b3 = lambda a, n: a.unsqueeze(2).to_broadcast([128, T4, n])
                dv(lambda e: e.tensor_reduce(out=gmax, in_=glt, axis=AX.X, op=ALU.max))
                dv(lambda e: e.tensor_tensor(out=ohg, in0=glt, in1=b3(gmax, NG), op=ALU.is_equal))
                dv(lambda e: e.tensor_tensor(out=eg, in0=glt, in1=b3(gmax, NG), op=ALU.subtract))
                op("act", lambda e: e.activation(out=eg, in_=eg, func=AF.Exp), r=RT, w=RT)
                dv(lambda e: e.tensor_reduce(out=sume, in_=eg, axis=AX.X, op=ALU.add))
                dv(lambda e: e.reciprocal(out=pgr, in_=sume))
                dv(lambda e: e.tensor_scalar(out=pen, in0=ohg, scalar1=-1.0, scalar2=BIG, op0=ALU.add, op1=ALU.mult))
                dv(lambda e: e.tensor_tensor(out=mm.rearrange("p t (g x) -> p (t g) x", x=EPG),
                                             in0=elt.rearrange("p t (g x) -> p (t g) x", x=EPG),
                                             in1=pen.rearrange("p t g -> p (t g)").unsqueeze(2).to_broadcast([128, T4 * NG, EPG]), op=ALU.add))
                dv(lambda e: e.tensor_reduce(out=m1, in_=mm, axis=AX.X, op=ALU.max))
                dv(lambda e: e.tensor_tensor(out=oh1, in0=mm, in1=b3(m1, NE), op=ALU.is_equal))
                dv(lambda e: e.scalar_tensor_tensor(out=mm2, in0=oh1, scalar=-BIG, in1=mm, op0=ALU.mult, op1=ALU.add))
                dv(lambda e: e.tensor_reduce(out=m2, in_=mm2, axis=AX.X, op=ALU.max))
                dv(lambda e: e.tensor_tensor(out=oh2, in0=mm2, in1=b3(m2, NE), op=ALU.is_equal))
                dv(lambda e: e.tensor_tensor(out=dd, in0=m2, in1=m1, op=ALU.subtract))
                op("act", lambda e: e.activation(out=e2, in_=dd, func=AF.Exp), r=RT, w=RT)
                dv(lambda e: e.tensor_scalar(out=e2, in0=e2, scalar1=1.0, scalar2=None, op0=ALU.add))
                dv(lambda e: e.reciprocal(out=e2, in_=e2))
                dv(lambda e: e.tensor_tensor(out=ga, in0=e2, in1=pgr, op=ALU.mult))
                dv(lambda e: e.tensor_tensor(out=gb, in0=pgr, in1=ga, op=ALU.subtract))
                op("dve", lambda e: e.tensor_copy(out=gates[:, ti0:ti0 + T4, 0], in_=ga), r=RT, w=["gates"])
                op("dve", lambda e: e.tensor_copy(out=gates[:, ti0:ti0 + T4, 1], in_=gb), r=RT, w=["gates"])
                dv(lambda e: e.tensor_tensor(out=tmpe, in0=oh1, in1=oh2, op=ALU.add))
                op("dve", lambda e: e.tensor_copy(out=A_all[:, ti0:ti0 + T4, :], in_=tmpe), r=RT, w=[f"Aq{q}"])
                psR, prR = bank()
                for tt in range(TPQ):
                    for pt in range(tt):
                        op("pe", lambda e, pt=pt: e.matmul(psR[:, tt * NE:(tt + 1) * NE], lhsT=onesb[:], rhs=A_all[:, ti0 + pt, :],
                                                           start=(pt == 0), stop=False), r=["onesb", f"Aq{q}"], w=[prR])
                    op("pe", lambda e: e.matmul(psR[:, tt * NE:(tt + 1) * NE], lhsT=triub[:], rhs=A_all[:, ti0 + tt, :],
                                                start=(tt == 0), stop=True), r=["triub", f"Aq{q}"], w=[prR])
                psC, prC = bank()
                for tt in range(TPQ):
                    op("pe", lambda e: e.matmul(psC[:, :NE], lhsT=onesb[:], rhs=A_all[:, ti0 + tt, :], start=(tt == 0), stop=(tt == TPQ - 1)),
                       r=["onesb", f"Aq{q}"], w=[prC])
                dv(lambda e: e.tensor_tensor(out=slf, in0=psR[:, :T4 * NE].rearrange("p (t n) -> p t n", n=NE),
                                             in1=cntq[:, :].unsqueeze(1).to_broadcast([128, T4, NE]), op=ALU.add), [prR, "cntq"])
                dv(lambda e: e.tensor_scalar(out=slf, in0=slf, scalar1=float(C - 1), scalar2=None, op0=ALU.min))
                dv(lambda e: e.tensor_tensor(out=slf, in0=slf, in1=ecst[:, :].unsqueeze(1).to_broadcast([128, T4, NE]), op=ALU.add), ["ecst"])
                op("dve", lambda e: e.tensor_tensor(out=cntq[:, :], in0=cntq[:, :], in1=psC[:, :NE], op=ALU.add), r=RT + [prC, "cntq"], w=["cntq"])
                for kk, (oh, sf) in enumerate(((oh1, s0f), (oh2, s1f))):
                    dv(lambda e, oh=oh: e.tensor_tensor(out=tmpe, in0=slf, in1=oh, op=ALU.mult))
                    dv(lambda e, sf=sf: e.tensor_reduce(out=sf, in_=tmpe, axis=AX.X, op=ALU.add))
                    op("dve", lambda e, kk=kk, sf=sf: e.tensor_copy(out=slots_i[:, ti0:ti0 + T4, kk], in_=sf), r=RT, w=["slots_i"])
                si8 = slots_i[:, ti0:ti0 + T4, :].rearrange("p t k -> p (t k)")
                fiq = smi8[q % 2]
                ai8, bi8, fi8 = fiq[:, 0:2 * T4], fiq[:, 2 * T4:4 * T4], fiq[:, 4 * T4:6 * T4]
                FR = [f"smi8_{q % 2}"]
                op("dve", lambda e: e.tensor_scalar(out=ai8, in0=si8, scalar1=127, scalar2=LNEB, op0=ALU.bitwise_and,
                                                    op1=ALU.logical_shift_left), r=["slots_i"] + FR, w=FR)
                op("dve", lambda e: e.tensor_scalar(out=bi8, in0=si8, scalar1=7, scalar2=None, op0=ALU.logical_shift_right),
                   r=["slots_i"] + FR, w=FR)
                op("dve", lambda e: e.tensor_tensor(out=fi8, in0=ai8, in1=bi8, op=ALU.bitwise_or), r=FR, w=FR)
                for tt in range(TPQ):
                    for kk in range(2):
                        c = 2 * tt + kk
                        op("gq", lambda e, c=c, tt=tt: e.indirect_dma_start(out=tki_d, out_offset=bass.IndirectOffsetOnAxis(ap=fi8[:, c:c + 1], axis=0),
                                                                            in_=tokid[:, ti0 + tt:ti0 + tt + 1], in_offset=None),
                           r=FR + ["tokid", "tki_d"], w=[f"tki_{ti0 + tt}_{kk}"])
            pump_ada(99)
            T.barrier()

        op("sq", lambda e: e.dma_start(out=tokidx[:], in_=tki_d.rearrange("(p n) o -> p (n o)", p=128)), r=["tki_d"], w=["tokidx"])
        with contextlib.ExitStack() as s4:
            wgu = [sb(f"wgu{j}", [128, KD, 2 * DE], BF16, s4) for j in range(2)]
            wdn = [sb(f"wdn{j}", [128, KE, D], BF16, s4) for j in range(2)]
            hgT = [sb(f"hgT{j}", [128, KD, 128], BF16, s4) for j in range(2)]
            sg = [sb(f"sg{j}", [128, DEW], F32, s4) for j in range(2)]
            actb = [sb(f"actb{j}", [128, DE], BF16, s4) for j in range(2)]
            actT = [sb(f"actT{j}", [128, KE, 128], BF16, s4) for j in range(2)]
            ysb = [sb(f"ysb{j}", [128, D], BF16, s4) for j in range(2)]

            NHG = 4
            hg = [sb(f"hgr{j}", [128, D], BF16, s4) for j in range(NHG)]
            NBLK = NE * B
            psg, prg = psb[0], "ps0"
            psu, pru = psb[1], "ps1"
            dnb = [(psb[2], "ps2"), (psb[3], "ps3"), (psb[4], "ps4")]
            psT2 = psb[5][:].bitcast(BF16)
            dn_rr = [0]

            def load_expert(e_):
                j = e_ % 2
                op("gq", lambda e: e.dma_start(out=wgu[j][:], in_=wgu_d[e_]), w=[f"wgu{j}"])
                op("gq", lambda e: e.dma_start(out=wdn[j][:], in_=wdn_d[e_]), w=[f"wdn{j}"])

            def gather(i):
                jh = i % NHG
                op("gq", lambda e: e.indirect_dma_start(out=hg[jh][:, :], out_offset=None, in_=h2_d,
                                                        in_offset=bass.IndirectOffsetOnAxis(ap=tokidx[:, i:i + 1], axis=0),
                                                        bounds_check=BC[0], oob_is_err=False),
                   r=["tokidx", "h2_d"], w=[f"hgr{jh}"])

            def S1(i):
                jh, j = i % NHG, i % 2
                for k in range(KD):
                    op("pe", lambda e, k=k: e.transpose(out=psT[:, k * 128:(k + 1) * 128], in_=hg[jh][:, k * 128:(k + 1) * 128],
                                                        identity=identb[:]), r=[f"hgr{jh}", "identb"], w=["psT"])
                hk = KD // 2
                op("act", lambda e: e.copy(out=hgT[j][:, :hk, :].rearrange("p k t -> p (k t)"), in_=psT[:, :hk * 128]),
                   r=["psT"], w=[f"hgT{j}a"])
                op("dve", lambda e: e.tensor_copy(out=hgT[j][:, hk:, :].rearrange("p k t -> p (k t)"), in_=psT[:, hk * 128:KD * 128]),
                   r=["psT"], w=[f"hgT{j}b"])
                if i + NHG < NBLK:
                    gather(i + NHG)

            def S2(i):
                j, je = i % 2, (i // B) % 2
                for dg in range(NDG):
                    for (pp_, pr_, c0) in ((psg, prg, dg * DEW), (psu, pru, DE + dg * DEW)):
                        for k in range(KD):
                            op("pe", lambda e, k=k, pp_=pp_, c0=c0: e.matmul(pp_[:, :DEW], lhsT=hgT[j][:, k, :], rhs=wgu[je][:, k, c0:c0 + DEW],
                                                                              start=(k == 0), stop=(k == KD - 1)),
                               r=[f"hgT{j}a", f"hgT{j}b", f"wgu{je}"], w=[pr_])
                    op("act", lambda e: e.activation(out=sg[j][:], in_=psg[:, :DEW], func=AF.Silu), r=[prg], w=[f"sg{j}"])
                    op("dve", lambda e: e.tensor_tensor(out=actb[j][:, dg * DEW:(dg + 1) * DEW], in0=sg[j][:], in1=psu[:, :DEW], op=ALU.mult),
                       r=[f"sg{j}", pru], w=[f"actb{j}"])

            def S3a(i):
                j = i % 2
                for k in range(KE):
                    op("pe", lambda e, k=k: e.transpose(out=psT2[:, k * 128:(k + 1) * 128], in_=actb[j][:, k * 128:(k + 1) * 128],
                                                        identity=identb[:]), r=[f"actb{j}", "identb"], w=["ps5"])
                op("dve", lambda e: e.tensor_copy(out=actT[j][:].rearrange("p k t -> p (k t)"), in_=psT2[:, :KE * 128]),
                   r=["ps5"], w=[f"actT{j}"])

            def S3b(i):
                j, je = i % 2, (i // B) % 2
                for og in range(NOG):
                    ps, pr = dnb[dn_rr[0]]
                    dn_rr[0] = (dn_rr[0] + 1) % 3
                    for k in range(KE):
                        op("pe", lambda e, k=k: e.matmul(ps[:, :OGW], lhsT=actT[j][:, k, :], rhs=wdn[je][:, k, og * OGW:(og + 1) * OGW],
                                                         start=(k == 0), stop=(k == KE - 1)), r=[f"actT{j}", f"wdn{je}"], w=[pr])
                    if og % 2 == 0:
                        op("act", lambda e: e.copy(out=ysb[j][:, og * OGW:(og + 1) * OGW], in_=ps[:, :OGW]), r=[pr], w=[f"ysb{j}"])
                    else:
                        op("dve", lambda e: e.tensor_copy(out=ysb[j][:, og * OGW:(og + 1) * OGW], in_=ps[:, :OGW]), r=[pr], w=[f"ysb{j}"])
                op("sq", lambda e: e.dma_start(out=y_d[i * 128:(i + 1) * 128, :], in_=ysb[j][:]), r=[f"ysb{j}"], w=["y_d"])

            _bcr = nc.gpsimd.alloc_register("bcreg")
            nc.gpsimd.reg_mov(_bcr, S - 1)
            BC = [nc.gpsimd.snap(_bcr)]
            for jh in range(NHG):
                op("dve", lambda e, jh=jh: e.memset(hg[jh][:], 0.0), w=[f"hgr{jh}"])
            load_expert(0)
            for i in range(min(NHG, NBLK)):
                gather(i)
            S1(0)
            for i in range(NBLK):
                if i >= 1:
                    S3a(i - 1)
                if i + 1 < NBLK:
                    S1(i + 1)
                S2(i)
                if i >= 1:
                    S3b(i - 1)
                if i % B == 0 and i // B + 1 < NE:
                    load_expert(i // B + 1)
            S3a(NBLK - 1)
            S3b(NBLK - 1)
            T.barrier()

        with contextlib.ExitStack() as s5:
            ya = [sb(f"ya{j}", [128, D], BF16, s5) for j in range(2)]
            yb = [sb(f"yb{j}", [128, D], BF16, s5) for j in range(2)]
            x1t = [sb(f"x1t{j}", [128, D], F32, s5) for j in range(2)]
            ft = [sb(f"ft{j}", [128, D], F32, s5) for j in range(2)]
            junk = sb("junk", [128, D], BF16, s5)
            gate2b = sb("gate2b", [128, D], F32, s5)
            gvfb = sb("gvfb", [128, D], F32, s5)
            shfb = sb("shfb", [128, D], F32, s5)
            for v, (t_, r_) in enumerate(((gate2b, "gate2b"), (shfb, "shfb"), (gvfb, "gvfb"))):
                op("sq", lambda e, v=v, t_=t_: e.dma_start(out=t_[:], in_=modv_d[v]), r=["modv_d"], w=[r_])
            for ti in range(NT):
                j = ti % 2
                op("gq", lambda e: e.indirect_dma_start(out=ya[j][:, :], out_offset=None, in_=y_d,
                                                        in_offset=bass.IndirectOffsetOnAxis(ap=slots_i[:, ti, 0:1], axis=0)),
                   r=["slots_i", "y_d"], w=[f"ya{j}"])
                op("gq", lambda e: e.indirect_dma_start(out=yb[j][:, :], out_offset=None, in_=y_d,
                                                        in_offset=bass.IndirectOffsetOnAxis(ap=slots_i[:, ti, 1:2], axis=0)),
                   r=["slots_i", "y_d"], w=[f"yb{j}"])
                op("sq", lambda e: e.dma_start(out=x1t[j][:], in_=x1_d[ti * 128:(ti + 1) * 128, :]), r=["x1_d"], w=[f"x1t{j}"])
                F = [f"ft{j}"]
                op("act", lambda e: e.activation(out=ft[j][:], in_=ya[j][:], func=AF.Identity, scale=gates[:, ti, 0:1]),
                   r=[f"ya{j}", "gates"], w=F)
                op("dve", lambda e: e.scalar_tensor_tensor(out=ft[j][:], in0=yb[j][:], scalar=gates[:, ti, 1:2], in1=ft[j][:],
                                                           op0=ALU.mult, op1=ALU.add), r=[f"yb{j}", "gates"] + F, w=F)
                op("dve", lambda e: e.tensor_tensor(out=ft[j][:], in0=ft[j][:], in1=gate2b[:], op=ALU.mult), r=F + ["gate2b"], w=F)
                op("dve", lambda e: e.tensor_tensor(out=ft[j][:], in0=ft[j][:], in1=x1t[j][:], op=ALU.add), r=F + [f"x1t{j}"], w=F)
                ssF = sm[:, 4:5]
                op("dve", lambda e: e.memset(ssF, 0.0), w=["ssF"])
                op("act", lambda e: e.activation(out=junk[:], in_=ft[j][:], func=AF.Square, accum_out=ssF), r=F + ["ssF"], w=["junk", "ssF"])
                rstd_from_ss(ssF, sm[:, 5:6], D, ["ssF"], ["rsF"])
                op("dve", lambda e: e.scalar_tensor_tensor(out=ft[j][:], in0=ft[j][:], scalar=sm[:, 5:6], in1=gvfb[:],
                                                           op0=ALU.mult, op1=ALU.mult), r=F + ["rsF", "gvfb"], w=F)
                op("dve", lambda e: e.tensor_tensor(out=ft[j][:], in0=ft[j][:], in1=shfb[:], op=ALU.add), r=F + ["shfb"], w=F)
                op("sq", lambda e: e.dma_start(out=out_d[ti * 128:(ti + 1) * 128, :], in_=ft[j][:]), r=F, w=["out_d"])
            T.barrier()
    return nc


def _tile_w(w, cw):
    K_, N_ = w.shape
    return np.ascontiguousarray(w.reshape(K_ // 128, 128, N_ // cw, cw).transpose(2, 1, 0, 3), dtype=np.float32)


def host_shared(cfg, w_ada, b_ada, norm1_g, w_in, w_out, gmlp_w_s, gmlp_b_s, gmlp_v_gain, conv_w, conv_b, norm2_g,
                w_router_group, b_router_group, w_router_expert, b_router_expert, w_gate_up, w_down, w_ada_final,
                b_ada_final, norm_f_g, **_):
    D, S, BW, C = cfg["D"], cfg["S"], cfg["BW"], cfg["C"]
    NE = cfg["NG"] * cfg["EPG"]
    KD, NT, BWC = D // 128, S // 128, BW // 128
    CWA, OGW = min(512, D), min(512, D)
    f = lambda a: np.ascontiguousarray(a, dtype=np.float32)
    return {
        "w_ada": _tile_w(w_ada[0], CWA), "b_ada": f(b_ada[0]), "norm1_g": f(norm1_g[0]),
        "w_in": _tile_w(w_in[0], 512), "w_out": _tile_w(w_out[0], OGW),
        "wsT": f(gmlp_w_s[0].transpose(2, 0, 1).reshape(128, -1)),
        "b_s": f(gmlp_b_s[0].reshape(-1)), "v_gain": f(gmlp_v_gain[0].reshape(-1)),
        "conv_w_pk": f(conv_w[0].reshape(3, BWC, 128).transpose(2, 0, 1).reshape(128, -1)),
        "conv_b_pk": f(conv_b[0].reshape(BWC, 128).T),
        "norm2_g": f(norm2_g[0]),
        "wr": f(np.concatenate([w_router_group[0], w_router_expert[0]], axis=1)),
        "rb": f(np.concatenate([b_router_group[0], b_router_expert[0]])),
        "w_gate_up": f(w_gate_up[0].reshape(NE, KD, 128, -1).transpose(0, 2, 1, 3)),
        "w_down": f(w_down[0].reshape(NE, -1, 128, D).transpose(0, 2, 1, 3)),
        "w_ada_final": _tile_w(w_ada_final, CWA), "b_ada_final": f(b_ada_final), "norm_f_g": f(norm_f_g),
        "ident": np.eye(128, dtype=np.float32),
        "triu": np.triu(np.ones((128, 128), np.float32), 1),
        "triu0": np.triu(np.ones((128, 128), np.float32), 0),
        "ecst": np.tile((np.arange(NE) * C).astype(np.float32)[None, :], (128, 1)),
        "tokid": np.ascontiguousarray((np.arange(NT)[None, :] * 128 + np.arange(128)[:, None]).astype(np.int32)),
    }


def host_inputs(cfg, b, shared=None, **inputs):
    if shared is None:
        shared = host_shared(cfg, **inputs)
    KD = cfg["D"] // 128
    m = dict(shared)
    m["x"] = np.ascontiguousarray(inputs["x"][b], dtype=np.float32)
    m["c_pk"] = np.ascontiguousarray(np.asarray(inputs["c"][b], dtype=np.float32).reshape(KD, 128).T)
    return m


_NC_CACHE = {}


def kernel(**inputs):
    cfg = REAL
    inputs = {k: np.asarray(v) for k, v in inputs.items()}
    nb = inputs["x"].shape[0]
    if "nc" not in _NC_CACHE:
        _NC_CACHE["nc"] = build(cfg)
    nc = _NC_CACHE["nc"]
    shared = host_shared(cfg, **inputs)
    in_maps = [host_inputs(cfg, b, shared=shared, **inputs) for b in range(nb)]
    res = run_bass_kernel_spmd(nc, in_maps, core_ids=list(range(nb)))
    return np.stack([np.asarray(r["out"], dtype=np.float32) for r in res.results], axis=0)
```

```python
import contextlib
import numpy as np
import concourse.bass as bass
import concourse.mybir as mybir
from concourse.bass_utils import run_bass_kernel_spmd

F32 = mybir.dt.float32
BF16 = mybir.dt.bfloat16
I32 = mybir.dt.int32
AF = mybir.ActivationFunctionType
ALU = mybir.AluOpType
AX = mybir.AxisListType
EPS = 1e-6
BIG = 30000.0

REAL = dict(D=2048, S=2048, AW=1024, BW=1024, NG=4, EPG=8, DE=512, C=512, TN=512)


class _Stream:
    def __init__(self, name, eng, sems, inc):
        self.name, self.eng, self.sems, self.inc = name, eng, sems, inc
        self.cnt = [0] * len(sems)
        self.rr = 0


class _Eng:
    def __init__(self, name, h):
        self.name, self.h, self.seen = name, h, {}


class Trk:
    def __init__(self, nc, es, ndma=8):
        mk = lambda n: es.enter_context(nc.semaphore(n))
        self.E = {n: _Eng(n, h) for n, h in [("pe", nc.tensor), ("act", nc.scalar), ("dve", nc.vector),
                                             ("pool", nc.gpsimd), ("sp", nc.sync)]}
        self.S = {}
        for n in ["pe", "act", "dve", "pool"]:
            self.S[n] = _Stream(n, self.E[n], [mk("c_" + n)], 1)
        self.S["sq"] = _Stream("sq", self.E["sp"], [mk(f"sq{i}") for i in range(ndma)], 16)
        self.S["gq"] = _Stream("gq", self.E["pool"], [mk(f"gq{i}") for i in range(ndma)], 16)
        self.lastw = {}
        self.rds = {}
        self.nwait = 0

    def _wait(self, eng, tok):
        st, idx, val = tok
        key = (st.name, idx)
        if eng.seen.get(key, 0) >= val:
            return
        eng.h.wait_ge(st.sems[idx], val)
        eng.seen[key] = val
        self.nwait += 1

    def op(self, sname, fn, r=(), w=()):
        st = self.S[sname]
        eng = st.eng
        deps = []
        for reg in r:
            t = self.lastw.get(reg)
            if t:
                deps.append(t)
        for reg in w:
            t = self.lastw.get(reg)
            if t:
                deps.append(t)
            for (sn, idx), val in self.rds.get(reg, {}).items():
                deps.append((self.S[sn], idx, val))
        if st.inc == 16:
            idx = st.rr
            st.rr = (st.rr + 1) % len(st.sems)
            if st.cnt[idx] > 0:
                deps.append((st, idx, st.cnt[idx]))
        else:
            idx = 0
        for t in deps:
            if t[0] is st and sname == "pe":
                continue
            self._wait(eng, t)
        ins = fn(eng.h)
        st.cnt[idx] += st.inc
        ins.then_inc(st.sems[idx], st.inc)
        tok = (st, idx, st.cnt[idx])
        for reg in w:
            self.lastw[reg] = tok
            self.rds[reg] = {}
        for reg in r:
            if reg not in w:
                self.rds.setdefault(reg, {})[(sname, idx)] = tok[2]
        return tok

    def barrier(self):
        for eng in self.E.values():
            for st in self.S.values():
                for idx, c in enumerate(st.cnt):
                    if c > 0:
                        self._wait(eng, (st, idx, c))
        self.lastw = {}
        self.rds = {}


def build(cfg, gelu_mode="tanh"):
    D, S, AW, BW = cfg["D"], cfg["S"], cfg["AW"], cfg["BW"]
    NG, EPG, DE, C, TN = cfg["NG"], cfg["EPG"], cfg["DE"], cfg["C"], cfg["TN"]
    H = AW // 128
    NE = NG * EPG
    NR = NG + NE
    KD = D // 128
    NT = S // 128
    TPQ = TN // 128
    NQ = S // TN
    KM = (AW + BW) // 128
    ICOLS = 2 * AW + 3 * BW
    BWC = BW // 128
    OGW = min(512, D)
    NOG = D // OGW
    VW = min(512, AW)
    NVG = AW // VW
    HPG = VW // 128
    CGW = min(512, BW)
    NCG = BW // CGW
    CPG = CGW // 128
    B = C // 128
    NEB = NE * B
    NSLOT = NE * C
    DEW = min(512, DE)
    NDG = DE // DEW
    KE = DE // 128
    KMAX = max(KD, KM)
    CWA = min(512, D)
    NCA = D // CWA
    KPC = CWA // 128
    assert VW == 512 and CGW == 512

    nc = bass.Bass("TRN2", target_bir_lowering=False)

    def din(name, shape, dt=F32):
        return nc.dram_tensor(name, list(shape), dt, kind="ExternalInput").ap()

    x_d = din("x", [S, D])
    c_d = din("c_pk", [128, KD])
    w_ada_d = din("w_ada", [6 * NCA, 128, KD, CWA])
    b_ada_d = din("b_ada", [6 * D])
    n1g_d = din("norm1_g", [D])
    w_in_d = din("w_in", [ICOLS // 512, 128, KD, 512])
    w_out_d = din("w_out", [NOG, 128, KM, OGW])
    wsT_d = din("wsT", [128, H * 128])
    bs_d = din("b_s", [H * 128])
    vg_d = din("v_gain", [AW])
    cw_d = din("conv_w_pk", [128, 3 * BWC])
    cbias_d = din("conv_b_pk", [128, BWC])
    n2g_d = din("norm2_g", [D])
    wr_d = din("wr", [D, NR])
    rb_d = din("rb", [NR])
    wgu_d = din("w_gate_up", [NE, 128, KD, 2 * DE])
    wdn_d = din("w_down", [NE, 128, KE, D])
    w_adaf_d = din("w_ada_final", [2 * NCA, 128, KD, CWA])
    b_adaf_d = din("b_ada_final", [2 * D])
    nfg_d = din("norm_f_g", [D])
    ident_d = din("ident", [128, 128])
    triu_d = din("triu", [128, 128])
    triu0_d = din("triu0", [128, 128])
    ecst_d = din("ecst", [128, NE])
    tokid_d = din("tokid", [128, NT], I32)
    out_d = nc.dram_tensor("out", [S, D], F32, kind="ExternalOutput").ap()
    x1_d = nc.dram_tensor("x1_scr", [S, D], F32, kind="Internal").ap()
    h2_d = nc.dram_tensor("h2_scr", [S, D], BF16, kind="Internal").ap()
    y_d = nc.dram_tensor("y_scr", [NSLOT, D], BF16, kind="Internal").ap()
    tki_d = nc.dram_tensor("tki_scr", [128 * NEB, 1], I32, kind="Internal").ap()
    modv_d = nc.dram_tensor("modv_scr", [3, 128, D], F32, kind="Internal").ap()

    es = contextlib.ExitStack()
    with es:
        T = Trk(nc, es)
        _uid = [0]

        def sb(name, shape, dt=F32, stack=es):
            _uid[0] += 1
            return stack.enter_context(nc.sbuf_tensor(f"{name}_{_uid[0]}", list(shape), dt))
        identf = sb("identf", [128, 128])
        identb = sb("identb", [128, 128], BF16)
        triub = sb("triub", [128, 128], BF16)
        onesb = sb("onesb", [128, 128], BF16)
        ecst = sb("ecst_t", [128, NE])
        tokid = sb("tokid_t", [128, NT], I32)
        wmT = sb("wmT", [128, H, 128], BF16)
        bsb = sb("bsb", [128, H, 128])
        gainb = sb("gainb", [128, AW])
        cw = sb("cw", [128, 3, BWC])
        cbias = sb("cbias", [128, BWC])
        wrb = sb("wrb", [128, KD, NR], BF16)
        rbb = sb("rbb", [128, NR])
        cact = sb("cact", [128, KD])
        cb = sb("cb", [128, KD, 128], BF16)
        gv1p = sb("gv1p", [128, KD])
        sh1p = sb("sh1p", [128, KD])
        gv2p = sb("gv2p", [128, KD])
        sh2p = sb("sh2p", [128, KD])
        A_all = sb("A_all", [128, NT, NE], BF16)
        slots_i = sb("slots_i", [128, NT, 2], I32)
        gates = sb("gates", [128, NT, 2])
        tokidx = sb("tokidx", [128, NEB], I32)
        carry = sb("carry", [128, BWC, 2])
        zt = sb("zt", [128, NEB], I32)
        sm = sb("sm", [128, 64])
        smi = sb("smi", [128, 16], I32)
        LNEB = NEB.bit_length() - 1
        assert (1 << LNEB) == NEB
        psT = es.enter_context(nc.psum_tensor("psT", [128, max(KD * 128, KE * 128, 1024)], BF16))
        NB = 6
        psb = [es.enter_context(nc.psum_tensor(f"ps{i}", [128, 512], F32)) for i in range(NB)]
        bank_rr = [0]

        def bank():
            i = bank_rr[0]
            bank_rr[0] = (i + 1) % NB
            return psb[i], f"ps{i}"

        op = T.op

        op("sq", lambda e: e.dma_start(out=identf[:], in_=ident_d), w=["identf"])
        op("gq", lambda e: e.dma_start(out=identb[:], in_=ident_d), w=["identb"])
        op("gq", lambda e: e.dma_start(out=triub[:], in_=triu_d), w=["triub"])
        op("sq", lambda e: e.dma_start(out=ecst[:], in_=ecst_d), w=["ecst"])
        op("sq", lambda e: e.dma_start(out=tokid[:], in_=tokid_d), w=["tokid"])
        with contextlib.ExitStack() as s0:
            wsf = sb("wsf", [128, H, 128], F32, s0)
            mk0 = sb("mk0", [128, 128], F32, s0)
            op("sq", lambda e: e.dma_start(out=wsf[:].rearrange("p h t -> p (h t)"), in_=wsT_d), w=["wsf"])
            op("sq", lambda e: e.dma_start(out=mk0[:], in_=triu0_d), w=["mk0"])
            op("dve", lambda e: e.tensor_tensor(out=wmT[:], in0=wsf[:], in1=mk0[:].unsqueeze(1).to_broadcast([128, H, 128]), op=ALU.mult),
               r=["wsf", "mk0"], w=["wmT"])
            T.barrier()
        op("sq", lambda e: e.dma_start(out=bsb[:].rearrange("p h t -> p (h t)"), in_=bs_d.partition_broadcast(128)), w=["bsb"])
        op("sq", lambda e: e.dma_start(out=gainb[:], in_=vg_d.partition_broadcast(128)), w=["gainb"])
        op("sq", lambda e: e.dma_start(out=cw[:].rearrange("p a b -> p (a b)"), in_=cw_d), w=["cw"])
        op("sq", lambda e: e.dma_start(out=cbias[:], in_=cbias_d), w=["cbias"])
        op("gq", lambda e: e.dma_start(out=wrb[:], in_=wr_d.rearrange("(k p) n -> p k n", p=128)), w=["wrb"])
        op("sq", lambda e: e.dma_start(out=rbb[:], in_=rb_d.partition_broadcast(128)), w=["rbb"])
        op("sq", lambda e: e.dma_start(out=cact[:], in_=c_d), w=["cact"])
        op("dve", lambda e: e.memset(onesb[:], 1.0), w=["onesb"])
        op("dve", lambda e: e.memset(carry[:], 0.0), w=["carry"])
        op("dve", lambda e: e.memset(zt[:], S), w=["zt"])
        op("sq", lambda e: e.dma_start(out=tki_d.rearrange("(p n) o -> p (n o)", p=128), in_=zt[:]), r=["zt"], w=["tki_d"])
        op("act", lambda e: e.activation(out=cact[:], in_=cact[:], func=AF.Silu), r=["cact"], w=["cact"])
        for k in range(KD):
            op("dve", lambda e, k=k: e.tensor_scalar(out=cb[:, k, :], in0=onesb[:], scalar1=cact[:, k:k + 1], scalar2=None,
                                                     op0=ALU.mult), r=["cact", "onesb"], w=["cb"])

        def rstd_from_ss(ss_ap, rs_ap, n, regs_r, regs_w):
            op("dve", lambda e: e.tensor_scalar(out=rs_ap, in0=ss_ap, scalar1=1.0 / n, scalar2=EPS, op0=ALU.mult, op1=ALU.add),
               r=regs_r, w=regs_w)
            op("act", lambda e: e.activation(out=rs_ap, in_=rs_ap, func=AF.Sqrt), r=regs_w, w=regs_w)
            op("dve", lambda e: e.reciprocal(out=rs_ap, in_=rs_ap), r=regs_w, w=regs_w)

        def gelu(out_ap, in_ap, tmp_ap, r, w, wt):
            if gelu_mode == "tanh":
                op("act", lambda e: e.activation(out=out_ap, in_=in_ap, func=AF.Gelu_apprx_tanh), r=r, w=w)
            else:
                op("act", lambda e: e.activation(out=tmp_ap, in_=in_ap, func=AF.Square), r=r, w=wt)
                op("dve", lambda e: e.tensor_scalar(out=tmp_ap, in0=tmp_ap, scalar1=0.044715 * 1.5957691216, scalar2=1.5957691216,
                                                    op0=ALU.mult, op1=ALU.add), r=wt, w=wt)
                op("dve", lambda e: e.tensor_tensor(out=tmp_ap, in0=tmp_ap, in1=in_ap, op=ALU.mult), r=list(wt) + list(r), w=wt)
                op("act", lambda e: e.activation(out=tmp_ap, in_=tmp_ap, func=AF.Sigmoid), r=wt, w=wt)
                op("dve", lambda e: e.tensor_tensor(out=out_ap, in0=tmp_ap, in1=in_ap, op=ALU.mult), r=list(wt) + list(r), w=w)

        def transpose_mod(src_ap, src_reg, sc_pp, sc_reg, bi_pp, bi_reg, dst_ap, dst_reg):
            for k in range(KD):
                op("pe", lambda e, k=k: e.transpose(out=psT[:, k * 128:(k + 1) * 128], in_=src_ap[:, k * 128:(k + 1) * 128],
                                                    identity=identb[:]), r=[src_reg, "identb"], w=["psT"])
            mt = modtmp_box[0]
            op("dve", lambda e: e.tensor_tensor(out=mt[:].rearrange("p (k t) -> p k t", t=128),
                                                in0=psT[:, :KD * 128].rearrange("p (k t) -> p k t", t=128),
                                                in1=sc_pp[:, :].unsqueeze(2).to_broadcast([128, KD, 128]), op=ALU.mult),
               r=["psT", sc_reg], w=["modtmp"])
            op("dve", lambda e: e.tensor_tensor(out=dst_ap, in0=mt[:].rearrange("p (k t) -> p k t", t=128),
                                                in1=bi_pp[:, :].unsqueeze(2).to_broadcast([128, KD, 128]), op=ALU.add),
               r=["modtmp", bi_reg], w=[dst_reg])

        modtmp_box = [None]

        def ada_vec(stack_tiles, wsrc, bsrc, col0, out_ap, out_reg):
            wa, bb = stack_tiles
            for ci in range(NCA):
                j = ada_rr[0]
                ada_rr[0] = (j + 1) % 2
                cs = slice(col0 + ci * CWA, col0 + (ci + 1) * CWA)
                op("gq", lambda e: e.dma_start(out=wa[j][:, :, :], in_=wsrc[(col0 // CWA) + ci]), w=[f"wa{j}"])
                op("sq", lambda e: e.dma_start(out=bb[j][:, :], in_=bsrc[cs].partition_broadcast(128)), w=[f"bb{j}"])
                ps, pr = bank()
                for k in range(KD):
                    op("pe", lambda e, k=k: e.matmul(ps[:, :CWA], lhsT=cb[:, k, :], rhs=wa[j][:, k, :], start=(k == 0), stop=(k == KD - 1)),
                       r=["cb", f"wa{j}"], w=[pr])
                op("dve", lambda e: e.tensor_tensor(out=out_ap[:, ci * CWA:(ci + 1) * CWA], in0=ps[:, :CWA], in1=bb[j][:, :], op=ALU.add),
                   r=[pr, f"bb{j}"], w=[out_reg])

        ada_rr = [0]

        def gain_mul(vec_ap, vec_reg, gsrc, tmp_ap, tmp_reg):
            op("sq", lambda e: e.dma_start(out=tmp_ap, in_=gsrc.partition_broadcast(128)), w=[tmp_reg])
            op("dve", lambda e: e.scalar_tensor_tensor(out=vec_ap, in0=vec_ap, scalar=1.0, in1=tmp_ap, op0=ALU.add, op1=ALU.mult),
               r=[vec_reg, tmp_reg], w=[vec_reg])

        def to_pp(vec_ap, vec_reg, pp_ap, pp_reg, tmp_ap, tmp_reg):
            op("dve", lambda e: e.tensor_tensor(out=tmp_ap.rearrange("p (k j) -> p k j", j=128),
                                                in0=vec_ap.rearrange("p (k j) -> p k j", j=128),
                                                in1=identf[:].unsqueeze(1).to_broadcast([128, KD, 128]), op=ALU.mult),
               r=[vec_reg, "identf"], w=[tmp_reg])
            op("dve", lambda e: e.tensor_reduce(out=pp_ap, in_=tmp_ap.rearrange("p (k j) -> p k j", j=128), axis=AX.X, op=ALU.add),
               r=[tmp_reg], w=[pp_reg])

        gate1b = sb("gate1b", [128, D])
        with contextlib.ExitStack() as s1:
            wa = [sb(f"wa{j}", [128, KD, CWA], BF16, s1) for j in range(2)]
            bb = [sb(f"bb{j}", [128, CWA], F32, s1) for j in range(2)]
            va = sb("ada_va", [128, D], F32, s1)
            vb = sb("ada_vb", [128, D], F32, s1)
            ada_vec((wa, bb), w_ada_d, b_ada_d, 0 * D, va[:], "ada_va")
            to_pp(va[:], "ada_va", sh1p[:], "sh1p", vb[:], "ada_vb")
            ada_vec((wa, bb), w_ada_d, b_ada_d, 1 * D, va[:], "ada_va")
            gain_mul(va[:], "ada_va", n1g_d, vb[:], "ada_vb")
            to_pp(va[:], "ada_va", gv1p[:], "gv1p", vb[:], "ada_vb")
            T.barrier()

        with contextlib.ExitStack() as s2:
            NWB = 3
            wbig = [sb(f"wbig{j}", [128, KMAX, 512], BF16, s2) for j in range(NWB)]
            wrr = [0]

            def load_w(src, nk, ncols):
                j = wrr[0]
                wrr[0] = (j + 1) % NWB
                op("gq", lambda e: e.dma_start(out=wbig[j][:, :nk, :ncols], in_=src), w=[f"wbig{j}"])
                return wbig[j], f"wbig{j}"

            h1T = sb("h1T", [128, KD, TN], BF16, s2)
            vn = sb("vn", [128, TPQ, AW], BF16, s2)
            yT = sb("yT", [128, KM, TN], BF16, s2)
            xq = sb("xq", [128, TPQ, D], F32, s2)
            xn = [sb(f"xn{j}", [128, D], BF16, s2) for j in range(2)]
            h2T = sb("h2T", [128, KD, 128], BF16, s2)
            vt = [sb(f"vt{j}", [128, VW], F32, s2) for j in range(2)]
            vsq = [sb(f"vsq{j}", [128, VW], F32, s2) for j in range(2)]
            uT = [sb(f"uT{j}", [128, TN], BF16, s2) for j in range(2)]
            gtmp = [sb(f"gtmp{j}", [128, 512], F32, s2) for j in range(2)] if gelu_mode != "tanh" else None
            gt = lambda j, n: gtmp[j][:, :n] if gtmp is not None else None
            modtmp_box[0] = sb("modtmp", [128, KD * 128], F32, s2)
            _ztmp1 = sb("ztmp0", [128, TN], F32, s2)
            ztmp = [_ztmp1, _ztmp1]
            cT = sb("cT", [128, CPG, TN], F32, s2)
            pre = sb("pre", [128, CPG, TN + 2], F32, s2)
            _cv1 = sb("cv0", [128, TN], F32, s2)
            cv = [_cv1, _cv1]
            _otmp1 = sb("otmp0", [128, OGW], F32, s2)
            otmp = [_otmp1, _otmp1]
            rt4 = [sb(f"rt4_{i}", [128, TPQ, NE], F32, s2) for i in range(10)]
            smr = sb("smr", [128, 32], F32, s2)
            smr2 = sb("smr2", [128, 11 * TPQ], F32, s2)
            smi8 = [sb(f"smi8_{i}", [128, 6 * TPQ], I32, s2) for i in range(2)]
            rtg = [sb(f"rtg_{i}", [128, TPQ, NG], F32, s2) for i in range(4)]
            cntq = sb("cntq", [128, NE], F32, s2)
            op("dve", lambda e: e.memset(cntq[:], 0.0), w=["cntq"])

            ada_q = []
            for kind, wsrc, bsrc, col0, gsrc in (("gate1", w_ada_d, b_ada_d, 2 * D, None), ("sh2", w_ada_d, b_ada_d, 3 * D, None),
                                                 ("gv2", w_ada_d, b_ada_d, 4 * D, n2g_d), ("gate2", w_ada_d, b_ada_d, 5 * D, None),
                                                 ("shf", w_adaf_d, b_adaf_d, 0, None), ("gvf", w_adaf_d, b_adaf_d, D, nfg_d)):
                for ci in range(NCA):
                    ada_q.append((kind, wsrc, bsrc, col0, gsrc, ci))
            N_EARLY = 3 * NCA

            def ada_load(spec):
                kind, wsrc, bsrc, col0, gsrc, ci = spec
                wa_, war = load_w(wsrc[(col0 // CWA) + ci], KD, CWA)
                return spec, wa_, war

            def ada_chunk(spec):
                ada_compute(*ada_load(spec))

            ada_pending = [None]

            def pump_tail(last_in_quarter):
                if ada_pending[0] is not None:
                    ada_compute(*ada_pending[0])
                    ada_pending[0] = None
                if not last_in_quarter and ada_done[0] < len(ada_q):
                    ada_pending[0] = ada_load(ada_q[ada_done[0]])
                    ada_done[0] += 1

            def ada_compute(spec, wa_, war):
                kind, wsrc, bsrc, col0, gsrc, ci = spec
                cs = slice(col0 + ci * CWA, col0 + (ci + 1) * CWA)
                ls = slice(ci * CWA, (ci + 1) * CWA)
                bbm, gbm, adt, adt2 = vt[0][:, :CWA], vt[1][:, :CWA], vsq[0][:, :CWA], vsq[1][:, :CWA]
                op("sq", lambda e: e.dma_start(out=bbm, in_=bsrc[cs].partition_broadcast(128)), w=["vt0"])
                ps, pr = bank()
                for k in range(KD):
                    op("pe", lambda e, k=k: e.matmul(ps[:, :CWA], lhsT=cb[:, k, :], rhs=wa_[:, k, :CWA], start=(k == 0), stop=(k == KD - 1)),
                       r=["cb", war], w=[pr])
                if kind == "gate1":
                    op("dve", lambda e: e.tensor_tensor(out=gate1b[:, ls], in0=ps[:, :CWA], in1=bbm, op=ALU.add), r=[pr, "vt0"], w=["gate1b"])
                    return
                op("dve", lambda e: e.tensor_tensor(out=adt, in0=ps[:, :CWA], in1=bbm, op=ALU.add), r=[pr, "vt0"], w=["vsq0"])
                if gsrc is not None:
                    op("sq", lambda e: e.dma_start(out=gbm, in_=gsrc[ls].partition_broadcast(128)), w=["vt1"])
                    op("dve", lambda e: e.scalar_tensor_tensor(out=adt, in0=adt, scalar=1.0, in1=gbm, op0=ALU.add, op1=ALU.mult),
                       r=["vsq0", "vt1"], w=["vsq0"])
                if kind in ("sh2", "gv2"):
                    pp_t, pp_r = (sh2p, "sh2p") if kind == "sh2" else (gv2p, "gv2p")
                    op("dve", lambda e: e.tensor_tensor(out=adt2.rearrange("p (k j) -> p k j", j=128),
                                                        in0=adt.rearrange("p (k j) -> p k j", j=128),
                                                        in1=identf[:].unsqueeze(1).to_broadcast([128, KPC, 128]), op=ALU.mult),
                       r=["vsq0", "identf"], w=["vsq1"])
                    op("dve", lambda e: e.tensor_reduce(out=pp_t[:, ci * KPC:(ci + 1) * KPC], in_=adt2.rearrange("p (k j) -> p k j", j=128),
                                                        axis=AX.X, op=ALU.add), r=["vsq1"], w=[pp_r])
                else:
                    v = {"gate2": 0, "shf": 1, "gvf": 2}[kind]
                    op("sq", lambda e: e.dma_start(out=modv_d[v, :, ls], in_=adt), r=["vsq0"], w=["modv_d"])

            ada_done = [0]

            def pump_ada(n, upto=None):
                lim = len(ada_q) if upto is None else upto
                while n > 0 and ada_done[0] < lim:
                    ada_chunk(ada_q[ada_done[0]])
                    ada_done[0] += 1
                    n -= 1

            for q in range(NQ):
                for tt in range(TPQ):
                    ti = q * TPQ + tt
                    xr = f"xq{tt}"
                    op("sq", lambda e: e.dma_start(out=xq[:, tt, :], in_=x_d[ti * 128:(ti + 1) * 128, :]), w=[xr])
                    j = ti % 2
                    ssA = sm[:, 0:1]
                    op("dve", lambda e: e.memset(ssA, 0.0), w=["ssA"])
                    op("act", lambda e: e.activation(out=xn[j][:], in_=xq[:, tt, :], func=AF.Square, accum_out=ssA),
                       r=[xr, "ssA"], w=[f"xn{j}", "ssA"])
                    rstd_from_ss(ssA, sm[:, 1:2], D, ["ssA"], ["rsA"])
                    op("dve", lambda e: e.tensor_scalar(out=xn[j][:], in0=xq[:, tt, :], scalar1=sm[:, 1:2], scalar2=None, op0=ALU.mult),
                       r=[xr, "rsA"], w=[f"xn{j}"])
                    transpose_mod(xn[j], f"xn{j}", gv1p, "gv1p", sh1p, "sh1p", h1T[:, :, tt * 128:(tt + 1) * 128], "h1T")
                for vg in range(NVG):
                    wv, wvr = load_w(w_in_d[(AW + vg * VW) // 512], KD, VW)
                    for tt in range(TPQ):
                        ps, pr = bank()
                        for k in range(KD):
                            op("pe", lambda e, k=k: e.matmul(ps[:, :VW], lhsT=h1T[:, k, tt * 128:(tt + 1) * 128], rhs=wv[:, k, :VW],
                                                             start=(k == 0), stop=(k == KD - 1)), r=["h1T", wvr], w=[pr])
                        j = tt % 2
                        gelu(vt[j][:], ps[:, :VW], gt(j, VW), [pr], [f"vt{j}"], [f"gtmp{j}"])
                        op("dve", lambda e: e.tensor_tensor(out=vsq[j][:], in0=vt[j][:], in1=vt[j][:], op=ALU.mult), r=[f"vt{j}"], w=[f"vsq{j}"])
                        vss = smr[:, 0:HPG]
                        op("dve", lambda e: e.tensor_reduce(out=vss, in_=vsq[j][:].rearrange("p (h d) -> p h d", d=128), axis=AX.X, op=ALU.add),
                           r=[f"vsq{j}"], w=["vss"])
                        rstd_from_ss(vss, smr[:, 8:8 + HPG], 128, ["vss"], ["vrs"])
                        op("dve", lambda e: e.tensor_tensor(out=vsq[j][:].rearrange("p (h d) -> p h d", d=128),
                                                            in0=vt[j][:].rearrange("p (h d) -> p h d", d=128),
                                                            in1=smr[:, 8:8 + HPG].unsqueeze(2).to_broadcast([128, HPG, 128]), op=ALU.mult),
                           r=[f"vt{j}", "vrs"], w=[f"vsq{j}"])
                        op("dve", lambda e: e.tensor_tensor(out=vn[:, tt, vg * VW:(vg + 1) * VW], in0=vsq[j][:],
                                                            in1=gainb[:, vg * VW:(vg + 1) * VW], op=ALU.mult),
                           r=[f"vsq{j}", "gainb"], w=["vn"])
                for ug in range(NVG):
                    pump_ada(2, upto=N_EARLY)
                    wu, wur = load_w(w_in_d[(ug * VW) // 512], KD, VW)
                    for hh in range(HPG):
                        h = ug * HPG + hh
                        ps, pr = bank()
                        for k in range(KD):
                            op("pe", lambda e, k=k: e.matmul(ps[:, :TN], lhsT=wu[:, k, hh * 128:(hh + 1) * 128], rhs=h1T[:, k, :],
                                                             start=(k == 0), stop=(k == KD - 1)), r=["h1T", wur], w=[pr])
                        j = h % 2
                        gelu(uT[j][:], ps[:, :TN], gt(j, TN), [pr], [f"uT{j}"], [f"gtmp{j}"])
                        ps2, pr2 = bank()
                        for c in range(TPQ):
                            op("pe", lambda e, c=c: e.matmul(ps2[:, c * 128:(c + 1) * 128], lhsT=vn[:, c, h * 128:(h + 1) * 128],
                                                             rhs=wmT[:, h, :], start=True, stop=True), r=["vn", "wmT"], w=[pr2])
                        op("dve", lambda e: e.tensor_tensor(out=ztmp[j][:].rearrange("p (c t) -> p c t", t=128),
                                                            in0=ps2[:, :TN].rearrange("p (c t) -> p c t", t=128),
                                                            in1=bsb[:, h, :].unsqueeze(1).to_broadcast([128, TPQ, 128]), op=ALU.add),
                           r=[pr2, "bsb"], w=["ztmp0"])
                        op("dve", lambda e: e.tensor_tensor(out=yT[:, h, :], in0=ztmp[j][:], in1=uT[j][:], op=ALU.mult),
                           r=["ztmp0", f"uT{j}"], w=[f"yT{h}"])
                for cg in range(NCG):
                    base = 2 * AW
                    pump_ada(2, upto=N_EARLY)
                    wc, wcr = load_w(w_in_d[(base + BW + cg * CGW) // 512], KD, CGW)
                    for jx in range(CPG):
                        ps, pr = bank()
                        for k in range(KD):
                            op("pe", lambda e, k=k: e.matmul(ps[:, :TN], lhsT=wc[:, k, jx * 128:(jx + 1) * 128], rhs=h1T[:, k, :],
                                                             start=(k == 0), stop=(k == KD - 1)), r=["h1T", wcr], w=[pr])
                        op("act", lambda e: e.copy(out=cT[:, jx, :], in_=ps[:, :TN]), r=[pr], w=[f"cT{jx}"])
                    pump_ada(2, upto=N_EARLY)
                    wx, wxr = load_w(w_in_d[(base + 2 * BW + cg * CGW) // 512], KD, CGW)
                    for jx in range(CPG):
                        jj = cg * CPG + jx
                        ps, pr = bank()
                        for k in range(KD):
                            op("pe", lambda e, k=k: e.matmul(ps[:, :TN], lhsT=wx[:, k, jx * 128:(jx + 1) * 128], rhs=h1T[:, k, :],
                                                             start=(k == 0), stop=(k == KD - 1)), r=["h1T", wxr], w=[pr])
                        op("dve", lambda e: e.tensor_copy(out=pre[:, jx, 0:2], in_=carry[:, jj, :]), r=["carry"], w=[f"pre{jx}"])
                        op("dve", lambda e: e.tensor_tensor(out=pre[:, jx, 2:TN + 2], in0=ps[:, :TN], in1=cT[:, jx, :], op=ALU.mult),
                           r=[pr, f"cT{jx}"], w=[f"pre{jx}"])
                        op("dve", lambda e: e.tensor_copy(out=carry[:, jj, :], in_=pre[:, jx, TN:TN + 2]), r=[f"pre{jx}"], w=["carry"])
                    pump_ada(2, upto=N_EARLY)
                    wb, wbr = load_w(w_in_d[(base + cg * CGW) // 512], KD, CGW)
                    for jx in range(CPG):
                        jj = cg * CPG + jx
                        j = jj % 2
                        ps, pr = bank()
                        for k in range(KD):
                            op("pe", lambda e, k=k: e.matmul(ps[:, :TN], lhsT=wb[:, k, jx * 128:(jx + 1) * 128], rhs=h1T[:, k, :],
                                                             start=(k == 0), stop=(k == KD - 1)), r=["h1T", wbr], w=[pr])
                        op("dve", lambda e: e.tensor_scalar(out=cv[j][:], in0=pre[:, jx, 0:TN], scalar1=cw[:, 0, jj:jj + 1], scalar2=None,
                                                            op0=ALU.mult), r=[f"pre{jx}", "cw"], w=["cv0"])
                        for kk in (1, 2):
                            op("dve", lambda e, kk=kk: e.scalar_tensor_tensor(out=cv[j][:], in0=pre[:, jx, kk:kk + TN],
                                                                              scalar=cw[:, kk, jj:jj + 1], in1=cv[j][:],
                                                                              op0=ALU.mult, op1=ALU.add),
                               r=[f"pre{jx}", "cw", "cv0"], w=["cv0"])
                        op("dve", lambda e: e.scalar_tensor_tensor(out=yT[:, H + jj, :], in0=cv[j][:], scalar=cbias[:, jj:jj + 1],
                                                                   in1=ps[:, :TN], op0=ALU.add, op1=ALU.mult),
                           r=["cv0", "cbias", pr], w=[f"yT{H + jj}"])
                pump_ada(99, upto=N_EARLY)
                yregs = [f"yT{k}" for k in range(KM)]
                for og in range(NOG):
                    wo, wor = load_w(w_out_d[og], KM, OGW)
                    for tt in range(TPQ):
                        ps, pr = bank()
                        for k in range(KM):
                            op("pe", lambda e, k=k: e.matmul(ps[:, :OGW], lhsT=yT[:, k, tt * 128:(tt + 1) * 128], rhs=wo[:, k, :OGW],
                                                             start=(k == 0), stop=(k == KM - 1)), r=[yregs[k], wor], w=[pr])
                        j = tt % 2
                        xr = f"xq{tt}"
                        op("dve", lambda e: e.tensor_tensor(out=otmp[j][:], in0=ps[:, :OGW], in1=gate1b[:, og * OGW:(og + 1) * OGW], op=ALU.mult),
                           r=[pr, "gate1b"], w=["otmp0"])
                        op("dve", lambda e: e.tensor_tensor(out=xq[:, tt, og * OGW:(og + 1) * OGW], in0=otmp[j][:],
                                                            in1=xq[:, tt, og * OGW:(og + 1) * OGW], op=ALU.add),
                           r=["otmp0", xr], w=[xr])
                ti0 = q * TPQ
                T4 = TPQ
                ssq, rsq = smr[:, 0:T4], smr[:, 4:4 + T4]
                RT = ["rt"]
                op("dve", lambda e: e.memset(ssq, 0.0), w=["ssq"])
                for tt in range(TPQ):
                    ti = ti0 + tt
                    xr = f"xq{tt}"
                    pump_tail(tt == TPQ - 1)
                    op("sq", lambda e: e.dma_start(out=x1_d[ti * 128:(ti + 1) * 128, :], in_=xq[:, tt, :]), r=[xr], w=["x1_d"])
                    op("act", lambda e: e.activation(out=xn[tt % 2][:], in_=xq[:, tt, :], func=AF.Square, accum_out=ssq[:, tt:tt + 1]),
                       r=[xr, "ssq"], w=[f"xn{tt % 2}", "ssq"])
                rstd_from_ss(ssq, rsq, D, ["ssq"], ["rsq"])
                psL, prL = bank()
                for tt in range(TPQ):
                    ti = ti0 + tt
                    xr = f"xq{tt}"
                    j = tt % 2
                    op("dve", lambda e: e.tensor_scalar(out=xn[j][:], in0=xq[:, tt, :], scalar1=rsq[:, tt:tt + 1], scalar2=None, op0=ALU.mult),
                       r=[xr, "rsq"], w=[f"xn{j}"])
                    transpose_mod(xn[j], f"xn{j}", gv2p, "gv2p", sh2p, "sh2p", h2T[:, :, :], "h2T")
                    for k in range(KD):
                        op("pe", lambda e, k=k: e.matmul(psL[:, tt * NR:(tt + 1) * NR], lhsT=h2T[:, k, :], rhs=wrb[:, k, :],
                                                         start=(k == 0), stop=(k == KD - 1)), r=["h2T", "wrb"], w=[prL])
                    for k in range(KD):
                        op("pe", lambda e, k=k: e.transpose(out=psT[:, k * 128:(k + 1) * 128], in_=h2T[:, k, :], identity=identb[:]),
                           r=["h2T", "identb"], w=["psT"])
                    op("act", lambda e: e.copy(out=xn[j][:], in_=psT[:, :KD * 128]), r=["psT"], w=[f"xn{j}"])
                    op("sq", lambda e: e.dma_start(out=h2_d[ti * 128:(ti + 1) * 128, :], in_=xn[j][:]), r=[f"xn{j}"], w=["h2_d"])
                dv = lambda f, r_=(): op("dve", f, r=RT + list(r_), w=RT)
                glt, elt = rtg[0][:, :, :], rt4[1][:, :, :]
                psL3 = psL[:, :T4 * NR].rearrange("p (t n) -> p t n", n=NR)
                rbb3 = rbb[:, :].unsqueeze(1).to_broadcast([128, T4, NR])
                dv(lambda e: e.tensor_tensor(out=glt, in0=psL3[:, :, 0:NG], in1=rbb3[:, :, 0:NG], op=ALU.add), [prL, "rbb"])
                dv(lambda e: e.tensor_tensor(out=elt, in0=psL3[:, :, NG:NR], in1=rbb3[:, :, NG:NR], op=ALU.add), [prL, "rbb"])
                gmax, sume, pgr, m1, m2, dd, e2, ga, gb, s0f, s1f = (smr2[:, i * T4:(i + 1) * T4] for i in range(11))
                ohg, pen, eg = rtg[1][:, :, :], rtg[2][:, :, :], rtg[3][:, :, :]
                mm, oh1, mm2, oh2, slf, tmpe = rt4[5][:, :, :], rt4[6][:, :, :], rt4[7][:, :, :], rt4[8][:, :, :], rt4[9][:, :, :], rt4[2][:, :, :]
                b3 = lambda a, n: a.unsqueeze(2).to_broadcast([128, T4, n])
                dv(lambda e: e.tensor_reduce(out=gmax, in_=glt, axis=AX.X, op=ALU.max))
                dv(lambda e: e.tensor_tensor(out=ohg, in0=glt, in1=b3(gmax, NG), op=ALU.is_equal))
                dv(lambda e: e.tensor_tensor(out=eg, in0=glt, in1=b3(gmax, NG), op=ALU.subtract))
                op("act", lambda e: e.activation(out=eg, in_=eg, func=AF.Exp), r=RT, w=RT)
                dv(lambda e: e.tensor_reduce(out=sume, in_=eg, axis=AX.X, op=ALU.add))
                dv(lambda e: e.reciprocal(out=pgr, in_=sume))
                dv(lambda e: e.tensor_scalar(out=pen, in0=ohg, scalar1=-1.0, scalar2=BIG, op0=ALU.add, op1=ALU.mult))
                dv(lambda e: e.tensor_tensor(out=mm.rearrange("p t (g x) -> p (t g) x", x=EPG),
                                             in0=elt.rearrange("p t (g x) -> p (t g) x", x=EPG),
                                             in1=pen.rearrange("p t g -> p (t g)").unsqueeze(2).to_broadcast([128, T4 * NG, EPG]), op=ALU.add))
                dv(lambda e: e.tensor_reduce(out=m1, in_=mm, axis=AX.X, op=ALU.max))
                dv(lambda e: e.tensor_tensor(out=oh1, in0=mm, in1=b3(m1, NE), op=ALU.is_equal))
                dv(lambda e: e.scalar_tensor_tensor(out=mm2, in0=oh1, scalar=-BIG, in1=mm, op0=ALU.mult, op1=ALU.add))
                dv(lambda e: e.tensor_reduce(out=m2, in_=mm2, axis=AX.X, op=ALU.max))
                dv(lambda e: e.tensor_tensor(out=oh2, in0=mm2, in1=b3(m2, NE), op=ALU.is_equal))
                dv(lambda e: e.tensor_tensor(out=dd, in0=m2, in1=m1, op=ALU.subtract))
                op("act", lambda e: e.activation(out=e2, in_=dd, func=AF.Exp), r=RT, w=RT)
                dv(lambda e: e.tensor_scalar(out=e2, in0=e2, scalar1=1.0, scalar2=None, op0=ALU.add))
                dv(lambda e: e.reciprocal(out=e2, in_=e2))
                dv(lambda e: e.tensor_tensor(out=ga, in0=e2, in1=pgr, op=ALU.mult))
                dv(lambda e: e.tensor_tensor(out=gb, in0=pgr, in1=ga, op=ALU.subtract))
                op("dve", lambda e: e.tensor_copy(out=gates[:, ti0:ti0 + T4, 0], in_=ga), r=RT, w=["gates"])
                op("dve", lambda e: e.tensor_copy(out=gates[:, ti0:ti0 + T4, 1], in_=gb), r=RT, w=["gates"])
                dv(lambda e: e.tensor_tensor(out=tmpe, in0=oh1, in1=oh2, op=ALU.add))
                op("dve", lambda e: e.tensor_copy(out=A_all[:, ti0:ti0 + T4, :], in_=tmpe), r=RT, w=[f"Aq{q}"])
                psR, prR = bank()
                for tt in range(TPQ):
                    for pt in range(tt):
                        op("pe", lambda e, pt=pt: e.matmul(psR[:, tt * NE:(tt + 1) * NE], lhsT=onesb[:], rhs=A_all[:, ti0 + pt, :],
                                                           start=(pt == 0), stop=False), r=["onesb", f"Aq{q}"], w=[prR])
                    op("pe", lambda e: e.matmul(psR[:, tt * NE:(tt + 1) * NE], lhsT=triub[:], rhs=A_all[:, ti0 + tt, :],
                                                start=(tt == 0), stop=True), r=["triub", f"Aq{q}"], w=[prR])
                psC, prC = bank()
                for tt in range(TPQ):
                    op("pe", lambda e: e.matmul(psC[:, :NE], lhsT=onesb[:], rhs=A_all[:, ti0 + tt, :], start=(tt == 0), stop=(tt == TPQ - 1)),
                       r=["onesb", f"Aq{q}"], w=[prC])
                dv(lambda e: e.tensor_tensor(out=slf, in0=psR[:, :T4 * NE].rearrange("p (t n) -> p t n", n=NE),
                                             in1=cntq[:, :].unsqueeze(1).to_broadcast([128, T4, NE]), op=ALU.add), [prR, "cntq"])
                dv(lambda e: e.tensor_scalar(out=slf, in0=slf, scalar1=float(C - 1), scalar2=None, op0=ALU.min))
                dv(lambda e: e.tensor_tensor(out=slf, in0=slf, in1=ecst[:, :].unsqueeze(1).to_broadcast([128, T4, NE]), op=ALU.add), ["ecst"])
                op("dve", lambda e: e.tensor_tensor(out=cntq[:, :], in0=cntq[:, :], in1=psC[:, :NE], op=ALU.add), r=RT + [prC, "cntq"], w=["cntq"])
                for kk, (oh, sf) in enumerate(((oh1, s0f), (oh2, s1f))):
                    dv(lambda e, oh=oh: e.tensor_tensor(out=tmpe, in0=slf, in1=oh, op=ALU.mult))
                    dv(lambda e, sf=sf: e.tensor_reduce(out=sf, in_=tmpe, axis=AX.X, op=ALU.add))
                    op("dve", lambda e, kk=kk, sf=sf: e.tensor_copy(out=slots_i[:, ti0:ti0 + T4, kk], in_=sf), r=RT, w=["slots_i"])
                si8 = slots_i[:, ti0:ti0 + T4, :].rearrange("p t k -> p (t k)")
                fiq = smi8[q % 2]
                ai8, bi8, fi8 = fiq[:, 0:2 * T4], fiq[:, 2 * T4:4 * T4], fiq[:, 4 * T4:6 * T4]
                FR = [f"smi8_{q % 2}"]
                op("dve", lambda e: e.tensor_scalar(out=ai8, in0=si8, scalar1=127, scalar2=LNEB, op0=ALU.bitwise_and,
                                                    op1=ALU.logical_shift_left), r=["slots_i"] + FR, w=FR)
                op("dve", lambda e: e.tensor_scalar(out=bi8, in0=si8, scalar1=7, scalar2=None, op0=ALU.logical_shift_right),
                   r=["slots_i"] + FR, w=FR)
                op("dve", lambda e: e.tensor_tensor(out=fi8, in0=ai8, in1=bi8, op=ALU.bitwise_or), r=FR, w=FR)
                for tt in range(TPQ):
                    for kk in range(2):
                        c = 2 * tt + kk
                        op("gq", lambda e, c=c, tt=tt: e.indirect_dma_start(out=tki_d, out_offset=bass.IndirectOffsetOnAxis(ap=fi8[:, c:c + 1], axis=0),
                                                                            in_=tokid[:, ti0 + tt:ti0 + tt + 1], in_offset=None),
                           r=FR + ["tokid", "tki_d"], w=[f"tki_{ti0 + tt}_{kk}"])
            pump_ada(99)
            T.barrier()

        op("sq", lambda e: e.dma_start(out=tokidx[:], in_=tki_d.rearrange("(p n) o -> p (n o)", p=128)), r=["tki_d"], w=["tokidx"])
        with contextlib.ExitStack() as s4:
            wgu = [sb(f"wgu{j}", [128, KD, 2 * DE], BF16, s4) for j in range(2)]
            wdn = [sb(f"wdn{j}", [128, KE, D], BF16, s4) for j in range(2)]
            hgT = [sb(f"hgT{j}", [128, KD, 128], BF16, s4) for j in range(2)]
            sg = [sb(f"sg{j}", [128, DEW], F32, s4) for j in range(2)]
            actb = [sb(f"actb{j}", [128, DE], BF16, s4) for j in range(2)]
            actT = [sb(f"actT{j}", [128, KE, 128], BF16, s4) for j in range(2)]
            ysb = [sb(f"ysb{j}", [128, D], BF16, s4) for j in range(2)]

            NHG = 4
            hg = [sb(f"hgr{j}", [128, D], BF16, s4) for j in range(NHG)]
            NBLK = NE * B
            psg, prg = psb[0], "ps0"
            psu, pru = psb[1], "ps1"
            dnb = [(psb[2], "ps2"), (psb[3], "ps3"), (psb[4], "ps4")]
            psT2 = psb[5][:].bitcast(BF16)
            dn_rr = [0]

            def load_expert(e_):
                j = e_ % 2
                op("gq", lambda e: e.dma_start(out=wgu[j][:], in_=wgu_d[e_]), w=[f"wgu{j}"])
                op("gq", lambda e: e.dma_start(out=wdn[j][:], in_=wdn_d[e_]), w=[f"wdn{j}"])

            def gather(i):
                jh = i % NHG
                op("gq", lambda e: e.indirect_dma_start(out=hg[jh][:, :], out_offset=None, in_=h2_d,
                                                        in_offset=bass.IndirectOffsetOnAxis(ap=tokidx[:, i:i + 1], axis=0),
                                                        bounds_check=BC[0], oob_is_err=False),
                   r=["tokidx", "h2_d"], w=[f"hgr{jh}"])

            def S1(i):
                jh, j = i % NHG, i % 2
                for k in range(KD):
                    op("pe", lambda e, k=k: e.transpose(out=psT[:, k * 128:(k + 1) * 128], in_=hg[jh][:, k * 128:(k + 1) * 128],
                                                        identity=identb[:]), r=[f"hgr{jh}", "identb"], w=["psT"])
                hk = KD // 2
                op("act", lambda e: e.copy(out=hgT[j][:, :hk, :].rearrange("p k t -> p (k t)"), in_=psT[:, :hk * 128]),
                   r=["psT"], w=[f"hgT{j}a"])
                op("dve", lambda e: e.tensor_copy(out=hgT[j][:, hk:, :].rearrange("p k t -> p (k t)"), in_=psT[:, hk * 128:KD * 128]),
                   r=["psT"], w=[f"hgT{j}b"])
                if i + NHG < NBLK:
                    gather(i + NHG)

            def S2(i):
                j, je = i % 2, (i // B) % 2
                for dg in range(NDG):
                    for (pp_, pr_, c0) in ((psg, prg, dg * DEW), (psu, pru, DE + dg * DEW)):
                        for k in range(KD):
                            op("pe", lambda e, k=k, pp_=pp_, c0=c0: e.matmul(pp_[:, :DEW], lhsT=hgT[j][:, k, :], rhs=wgu[je][:, k, c0:c0 + DEW],
                                                                              start=(k == 0), stop=(k == KD - 1)),
                               r=[f"hgT{j}a", f"hgT{j}b", f"wgu{je}"], w=[pr_])
                    op("act", lambda e: e.activation(out=sg[j][:], in_=psg[:, :DEW], func=AF.Silu), r=[prg], w=[f"sg{j}"])
                    op("dve", lambda e: e.tensor_tensor(out=actb[j][:, dg * DEW:(dg + 1) * DEW], in0=sg[j][:], in1=psu[:, :DEW], op=ALU.mult),
                       r=[f"sg{j}", pru], w=[f"actb{j}"])

            def S3a(i):
                j = i % 2
                for k in range(KE):
                    op("pe", lambda e, k=k: e.transpose(out=psT2[:, k * 128:(k + 1) * 128], in_=actb[j][:, k * 128:(k + 1) * 128],
                                                        identity=identb[:]), r=[f"actb{j}", "identb"], w=["ps5"])
                op("dve", lambda e: e.tensor_copy(out=actT[j][:].rearrange("p k t -> p (k t)"), in_=psT2[:, :KE * 128]),
                   r=["ps5"], w=[f"actT{j}"])

            def S3b(i):
                j, je = i % 2, (i // B) % 2
                for og in range(NOG):
                    ps, pr = dnb[dn_rr[0]]
                    dn_rr[0] = (dn_rr[0] + 1) % 3
                    for k in range(KE):
                        op("pe", lambda e, k=k: e.matmul(ps[:, :OGW], lhsT=actT[j][:, k, :], rhs=wdn[je][:, k, og * OGW:(og + 1) * OGW],
                                                         start=(k == 0), stop=(k == KE - 1)), r=[f"actT{j}", f"wdn{je}"], w=[pr])
                    if og % 2 == 0:
                        op("act", lambda e: e.copy(out=ysb[j][:, og * OGW:(og + 1) * OGW], in_=ps[:, :OGW]), r=[pr], w=[f"ysb{j}"])
                    else:
                        op("dve", lambda e: e.tensor_copy(out=ysb[j][:, og * OGW:(og + 1) * OGW], in_=ps[:, :OGW]), r=[pr], w=[f"ysb{j}"])
                op("sq", lambda e: e.dma_start(out=y_d[i * 128:(i + 1) * 128, :], in_=ysb[j][:]), r=[f"ysb{j}"], w=["y_d"])

            _bcr = nc.gpsimd.alloc_register("bcreg")
            nc.gpsimd.reg_mov(_bcr, S - 1)
            BC = [nc.gpsimd.snap(_bcr)]
            for jh in range(NHG):
                op("dve", lambda e, jh=jh: e.memset(hg[jh][:], 0.0), w=[f"hgr{jh}"])
            load_expert(0)
            for i in range(min(NHG, NBLK)):
                gather(i)
            S1(0)
            for i in range(NBLK):
                if i >= 1:
                    S3a(i - 1)
                if i + 1 < NBLK:
                    S1(i + 1)
                S2(i)
                if i >= 1:
                    S3b(i - 1)
                if i % B == 0 and i // B + 1 < NE:
                    load_expert(i // B + 1)
            S3a(NBLK - 1)
            S3b(NBLK - 1)
            T.barrier()

        with contextlib.ExitStack() as s5:
            ya = [sb(f"ya{j}", [128, D], BF16, s5) for j in range(2)]
            yb = [sb(f"yb{j}", [128, D], BF16, s5) for j in range(2)]
            x1t = [sb(f"x1t{j}", [128, D], F32, s5) for j in range(2)]
            ft = [sb(f"ft{j}", [128, D], F32, s5) for j in range(2)]
            junk = sb("junk", [128, D], BF16, s5)
            gate2b = sb("gate2b", [128, D], F32, s5)
            gvfb = sb("gvfb", [128, D], F32, s5)
            shfb = sb("shfb", [128, D], F32, s5)
            for v, (t_, r_) in enumerate(((gate2b, "gate2b"), (shfb, "shfb"), (gvfb, "gvfb"))):
                op("sq", lambda e, v=v, t_=t_: e.dma_start(out=t_[:], in_=modv_d[v]), r=["modv_d"], w=[r_])
            def FX(ti):
                j = ti % 2
                op("gq", lambda e: e.indirect_dma_start(out=ya[j][:, :], out_offset=None, in_=y_d,
                                                        in_offset=bass.IndirectOffsetOnAxis(ap=slots_i[:, ti, 0:1], axis=0)),
                   r=["slots_i", "y_d"], w=[f"ya{j}"])
                op("gq", lambda e: e.indirect_dma_start(out=yb[j][:, :], out_offset=None, in_=y_d,
                                                        in_offset=bass.IndirectOffsetOnAxis(ap=slots_i[:, ti, 1:2], axis=0)),
                   r=["slots_i", "y_d"], w=[f"yb{j}"])
                op("sq", lambda e: e.dma_start(out=x1t[j][:], in_=x1_d[ti * 128:(ti + 1) * 128, :]), r=["x1_d"], w=[f"x1t{j}"])
                F = [f"ft{j}"]
                op("act", lambda e: e.activation(out=ft[j][:], in_=ya[j][:], func=AF.Identity, scale=gates[:, ti, 0:1]),
                   r=[f"ya{j}", "gates"], w=F)
                op("dve", lambda e: e.scalar_tensor_tensor(out=ft[j][:], in0=yb[j][:], scalar=gates[:, ti, 1:2], in1=ft[j][:],
                                                           op0=ALU.mult, op1=ALU.add), r=[f"yb{j}", "gates"] + F, w=F)
                op("dve", lambda e: e.tensor_tensor(out=ft[j][:], in0=ft[j][:], in1=gate2b[:], op=ALU.mult), r=F + ["gate2b"], w=F)
                op("dve", lambda e: e.tensor_tensor(out=ft[j][:], in0=ft[j][:], in1=x1t[j][:], op=ALU.add), r=F + [f"x1t{j}"], w=F)
                ssF = sm[:, 4 + 2 * j:5 + 2 * j]
                op("dve", lambda e: e.memset(ssF, 0.0), w=[f"ssF{j}"])
                op("act", lambda e: e.activation(out=junk[:], in_=ft[j][:], func=AF.Square, accum_out=ssF), r=F + [f"ssF{j}"],
                   w=["junk", f"ssF{j}"])

            def FY(ti):
                j = ti % 2
                F = [f"ft{j}"]
                ssF, rsF = sm[:, 4 + 2 * j:5 + 2 * j], sm[:, 5 + 2 * j:6 + 2 * j]
                op("dve", lambda e: e.scalar_tensor_tensor(out=ft[j][:], in0=ft[j][:], scalar=rsF, in1=gvfb[:],
                                                           op0=ALU.mult, op1=ALU.mult), r=F + [f"rsF{j}", "gvfb"], w=F)
                op("dve", lambda e: e.tensor_tensor(out=ft[j][:], in0=ft[j][:], in1=shfb[:], op=ALU.add), r=F + ["shfb"], w=F)
                op("sq", lambda e: e.dma_start(out=out_d[ti * 128:(ti + 1) * 128, :], in_=ft[j][:]), r=F, w=["out_d"])

            FX(0)
            for ti in range(NT):
                j = ti % 2
                rstd_from_ss(sm[:, 4 + 2 * j:5 + 2 * j], sm[:, 5 + 2 * j:6 + 2 * j], D, [f"ssF{j}"], [f"rsF{j}"])
                if ti + 1 < NT:
                    FX(ti + 1)
                FY(ti)
            T.barrier()
    return nc


def _tile_w(w, cw):
    K_, N_ = w.shape
    return np.ascontiguousarray(w.reshape(K_ // 128, 128, N_ // cw, cw).transpose(2, 1, 0, 3), dtype=np.float32)


def host_shared(cfg, w_ada, b_ada, norm1_g, w_in, w_out, gmlp_w_s, gmlp_b_s, gmlp_v_gain, conv_w, conv_b, norm2_g,
                w_router_group, b_router_group, w_router_expert, b_router_expert, w_gate_up, w_down, w_ada_final,
                b_ada_final, norm_f_g, **_):
    D, S, BW, C = cfg["D"], cfg["S"], cfg["BW"], cfg["C"]
    NE = cfg["NG"] * cfg["EPG"]
    KD, NT, BWC = D // 128, S // 128, BW // 128
    CWA, OGW = min(512, D), min(512, D)
    f = lambda a: np.ascontiguousarray(a, dtype=np.float32)
    return {
        "w_ada": _tile_w(w_ada[0], CWA), "b_ada": f(b_ada[0]), "norm1_g": f(norm1_g[0]),
        "w_in": _tile_w(w_in[0], 512), "w_out": _tile_w(w_out[0], OGW),
        "wsT": f(gmlp_w_s[0].transpose(2, 0, 1).reshape(128, -1)),
        "b_s": f(gmlp_b_s[0].reshape(-1)), "v_gain": f(gmlp_v_gain[0].reshape(-1)),
        "conv_w_pk": f(conv_w[0].reshape(3, BWC, 128).transpose(2, 0, 1).reshape(128, -1)),
        "conv_b_pk": f(conv_b[0].reshape(BWC, 128).T),
        "norm2_g": f(norm2_g[0]),
        "wr": f(np.concatenate([w_router_group[0], w_router_expert[0]], axis=1)),
        "rb": f(np.concatenate([b_router_group[0], b_router_expert[0]])),
        "w_gate_up": f(w_gate_up[0].reshape(NE, KD, 128, -1).transpose(0, 2, 1, 3)),
        "w_down": f(w_down[0].reshape(NE, -1, 128, D).transpose(0, 2, 1, 3)),
        "w_ada_final": _tile_w(w_ada_final, CWA), "b_ada_final": f(b_ada_final), "norm_f_g": f(norm_f_g),
        "ident": np.eye(128, dtype=np.float32),
        "triu": np.triu(np.ones((128, 128), np.float32), 1),
        "triu0": np.triu(np.ones((128, 128), np.float32), 0),
        "ecst": np.tile((np.arange(NE) * C).astype(np.float32)[None, :], (128, 1)),
        "tokid": np.ascontiguousarray((np.arange(NT)[None, :] * 128 + np.arange(128)[:, None]).astype(np.int32)),
    }


def host_inputs(cfg, b, shared=None, **inputs):
    if shared is None:
        shared = host_shared(cfg, **inputs)
    KD = cfg["D"] // 128
    m = dict(shared)
    m["x"] = np.ascontiguousarray(inputs["x"][b], dtype=np.float32)
    m["c_pk"] = np.ascontiguousarray(np.asarray(inputs["c"][b], dtype=np.float32).reshape(KD, 128).T)
    return m


_NC_CACHE = {}


def kernel(**inputs):
    cfg = REAL
    inputs = {k: np.asarray(v) for k, v in inputs.items()}
    nb = inputs["x"].shape[0]
    if "nc" not in _NC_CACHE:
        _NC_CACHE["nc"] = build(cfg)
    nc = _NC_CACHE["nc"]
    shared = host_shared(cfg, **inputs)
    in_maps = [host_inputs(cfg, b, shared=shared, **inputs) for b in range(nb)]
    res = run_bass_kernel_spmd(nc, in_maps, core_ids=list(range(nb)))
    return np.stack([np.asarray(r["out"], dtype=np.float32) for r in res.results], axis=0)
```
